# Optimizing a Trainium2 kernel written in Bass

```python
import math
import jax, jax.numpy as jnp
from jax import lax
import numpy as np

D_MODEL = 2048
BATCH = 1
SEQ = 8192
DEPTH = 1

D_MIX = D_MODEL
D_GMLP = D_MIX // 2
GMLP_GROUPS = 8
GMLP_GROUP_DIM = D_GMLP // GMLP_GROUPS
CHUNK = 128
D_ATTN = D_MIX - D_GMLP
HEAD_DIM = 128
N_HEADS = D_ATTN // HEAD_DIM
DILATED_BRANCHES = ((128, 1), (512, 4), (2048, 16))
MAX_DILATION = 16
BLK = 128
D_IN_PROJ = 2 * D_GMLP + 3 * D_ATTN
N_EXPERTS = 32
TOP_K = 4
D_EXPERT = D_MODEL
SWIGLU_LIMIT = 7.0
SWIGLU_ALPHA = 1.702
EXPERT_BLOCK = 128
LN_EPS = 1e-5
DEEPNORM_ALPHA = (2.0 * DEPTH) ** 0.25
DEEPNORM_BETA = (8.0 * DEPTH) ** -0.25

kernel_name = "hymba_gmlp_dilated_moe_block"


def layer_norm(x, g, b):
    xf = x.astype(jnp.float32)
    mu = jnp.mean(xf, axis=-1, keepdims=True)
    xc = xf - mu
    var = jnp.mean(xc * xc, axis=-1, keepdims=True)
    return (xc * lax.rsqrt(var + LN_EPS) * g + b).astype(x.dtype)


def gmlp_mixer(ua, va, ln_g, ln_b, w_s, b_s):
    B, S, _ = ua.shape
    n = S // CHUNK
    u = jax.nn.gelu(ua, approximate=False)
    v = jax.nn.gelu(va, approximate=False).reshape(B, n, CHUNK, GMLP_GROUPS, GMLP_GROUP_DIM)
    v = layer_norm(v, ln_g, ln_b)
    w = jnp.tril(w_s).astype(v.dtype)
    s = jnp.einsum('gts,bnsgc->bntgc', w, v) + b_s.T[None, None, :, :, None].astype(v.dtype)
    return u * s.reshape(B, S, D_GMLP)


def dilated_branch(q, k, v, slopes, window, dilation):
    B, H, Sp, hd = q.shape
    steps = window // dilation
    assert steps <= BLK
    L = Sp // dilation
    nb = L // BLK

    def to_res(t):
        return t.reshape(B, H, L, dilation, hd).transpose(0, 1, 3, 2, 4).reshape(B, H, dilation, nb, BLK, hd)

    qr, kr, vr = to_res(q), to_res(k), to_res(v)

    def with_prev(t):
        prev = jnp.pad(t[:, :, :, :-1], [(0, 0), (0, 0), (0, 0), (1, 0), (0, 0), (0, 0)])
        return jnp.concatenate([prev, t], axis=4)

    kb, vb = with_prev(kr), with_prev(vr)
    s = jnp.einsum('bhrnqc,bhrnkc->bhrnqk', qr, kb).astype(jnp.float32) * (hd ** -0.5)
    qi = jnp.arange(BLK)[:, None] + BLK
    ki = jnp.arange(2 * BLK)[None, :]
    step = qi - ki
    blk = jnp.arange(nb)[:, None, None]
    valid = (step >= 0) & (step <= steps) & ((blk > 0) | (ki >= BLK))
    dist = (step * dilation).astype(jnp.float32)
    s = s - slopes[None, :, None, None, None, None] * dist
    s = jnp.where(valid, s, -1e30)
    m = jnp.max(s, axis=-1, keepdims=True)
    p = jnp.exp(s - m)
    den = jnp.sum(p, axis=-1)
    o = jnp.einsum('bhrnqk,bhrnkc->bhrnqc', p.astype(vb.dtype), vb).astype(jnp.float32) / den[..., None]
    lse = m[..., 0] + jnp.log(den)
    o = o.reshape(B, H, dilation, L, hd).transpose(0, 1, 3, 2, 4).reshape(B, H, Sp, hd)
    lse = lse.reshape(B, H, dilation, L).transpose(0, 1, 3, 2).reshape(B, H, Sp)
    return o, lse


def dilated_attention(q, k, v):
    B, S, H, hd = q.shape
    span = MAX_DILATION * BLK
    Sp = -(-S // span) * span
    pad = [(0, 0), (0, Sp - S), (0, 0), (0, 0)]
    q, k, v = [jnp.pad(t, pad).transpose(0, 2, 1, 3) for t in (q, k, v)]
    slopes = jnp.exp2(-8.0 * jnp.arange(1, H + 1, dtype=jnp.float32) / H)
    outs, lses = [], []
    for window, dilation in DILATED_BRANCHES:
        o, lse = dilated_branch(q, k, v, slopes, window, dilation)
        outs.append(o)
        lses.append(lse)
    w = jax.nn.softmax(jnp.stack(lses, axis=0), axis=0)
    o = jnp.sum(w[..., None] * jnp.stack(outs, axis=0), axis=0)
    return o.transpose(0, 2, 1, 3)[:, :S].reshape(B, S, H * hd).astype(q.dtype)


def moe_ffn(h, w_router, b_router, w1, b1, w2, b2):
    B, S, D = h.shape
    N = B * S
    T = EXPERT_BLOCK
    xt = h.reshape(N, D)
    logits = (xt @ w_router + b_router).astype(jnp.float32)
    top_val, top_idx = lax.top_k(logits, TOP_K)
    gates = jax.nn.softmax(top_val, axis=-1)
    NK = N * TOP_K
    e_flat = top_idx.reshape(NK).astype(jnp.int32)
    g_flat = gates.reshape(NK)
    tok_flat = jnp.arange(NK, dtype=jnp.int32) // TOP_K
    order = jnp.argsort(e_flat)
    e_sorted, tok_sorted, g_sorted = e_flat[order], tok_flat[order], g_flat[order]
    counts = jnp.zeros((N_EXPERTS,), jnp.int32).at[e_flat].add(1)
    padded = (counts + T - 1) // T * T
    start = jnp.cumsum(counts) - counts
    pend = jnp.cumsum(padded)
    pstart = pend - padded
    dest = pstart[e_sorted] + jnp.arange(NK, dtype=jnp.int32) - start[e_sorted]
    cap = -(-NK // T) * T + N_EXPERTS * T
    n_blocks = cap // T
    buf_tok = jnp.full((cap,), N, jnp.int32).at[dest].set(tok_sorted)
    buf_gate = jnp.zeros((cap,), jnp.float32).at[dest].set(g_sorted)
    block_e = jnp.minimum(jnp.searchsorted(pend, jnp.arange(n_blocks, dtype=jnp.int32) * T, side='right'),
                          N_EXPERTS - 1)
    x_pad = jnp.concatenate([xt, jnp.zeros((1, D), xt.dtype)], axis=0)
    x_buf = x_pad[buf_tok].reshape(n_blocks, T, D)

    def run_block(args):
        xb, e = args
        h1 = xb @ w1[e] + b1[e]
        glu, lin = h1[:, :D_EXPERT], h1[:, D_EXPERT:]
        glu = jnp.minimum(glu, SWIGLU_LIMIT)
        lin = jnp.clip(lin, -SWIGLU_LIMIT, SWIGLU_LIMIT)
        act = glu * jax.nn.sigmoid(SWIGLU_ALPHA * glu) * (lin + 1.0)
        return act @ w2[e] + b2[e]

    out = lax.map(run_block, (x_buf, block_e)).reshape(cap, D)
    y = jnp.zeros((N + 1, D), jnp.float32).at[buf_tok].add(out.astype(jnp.float32) * buf_gate[:, None])[:N]
    return y.astype(h.dtype).reshape(B, S, D)


def setup_inputs(seed: int = 0) -> dict:
    key = jax.random.key(seed)
    ks = jax.random.split(key, 22)
    L = DEPTH

    def nrm(k, shape, scale):
        return jax.random.normal(k, shape, jnp.float32) * scale

    return {
        "x": nrm(ks[0], (BATCH, SEQ, D_MODEL), 1.0),
        "c": nrm(ks[1], (BATCH, D_MODEL), 1.0),
        "w_ada": nrm(ks[2], (L, D_MODEL, 6 * D_MODEL), 0.5 * D_MODEL ** -0.5),
        "b_ada": nrm(ks[3], (L, 6 * D_MODEL), 0.02),
        "w_in": nrm(ks[4], (L, D_MODEL, D_IN_PROJ), D_MODEL ** -0.5),
        "sgu_ln_g": 1.0 + nrm(ks[5], (L, GMLP_GROUPS, GMLP_GROUP_DIM), 0.02),
        "sgu_ln_b": nrm(ks[6], (L, GMLP_GROUPS, GMLP_GROUP_DIM), 0.02),
        "w_spatial": nrm(ks[7], (L, GMLP_GROUPS, CHUNK, CHUNK), 0.5 * CHUNK ** -0.5),
        "b_spatial": 1.0 + nrm(ks[8], (L, GMLP_GROUPS, CHUNK), 0.02),
        "w_o": nrm(ks[9], (L, D_MIX, D_MODEL), DEEPNORM_BETA * D_MIX ** -0.5),
        "ln1_g": 1.0 + nrm(ks[10], (L, D_MODEL), 0.02),
        "ln1_b": nrm(ks[11], (L, D_MODEL), 0.02),
        "w_router": nrm(ks[12], (L, D_MODEL, N_EXPERTS), D_MODEL ** -0.5),
        "b_router": nrm(ks[13], (L, N_EXPERTS), 0.01),
        "w_exp1": nrm(ks[14], (L, N_EXPERTS, D_MODEL, 2 * D_EXPERT), D_MODEL ** -0.5),
        "b_exp1": nrm(ks[15], (L, N_EXPERTS, 2 * D_EXPERT), 0.02),
        "w_exp2": nrm(ks[16], (L, N_EXPERTS, D_EXPERT, D_MODEL), DEEPNORM_BETA * D_EXPERT ** -0.5),
        "b_exp2": nrm(ks[17], (L, N_EXPERTS, D_MODEL), 0.02),
        "ln2_g": 1.0 + nrm(ks[18], (L, D_MODEL), 0.02),
        "ln2_b": nrm(ks[19], (L, D_MODEL), 0.02),
    }


def reference(x, c, w_ada, b_ada, w_in, sgu_ln_g, sgu_ln_b, w_spatial, b_spatial, w_o, ln1_g, ln1_b,
              w_router, b_router, w_exp1, b_exp1, w_exp2, b_exp2, ln2_g, ln2_b):
    B, S, D = x.shape
    splits = [D_GMLP, 2 * D_GMLP, 2 * D_GMLP + D_ATTN, 2 * D_GMLP + 2 * D_ATTN]
    for l in range(DEPTH):
        ada = jax.nn.silu(c) @ w_ada[l] + b_ada[l]
        shift1, scale1, gate1, shift2, scale2, gate2 = jnp.split(ada[:, None, :], 6, axis=-1)

        h = x * (1.0 + scale1) + shift1
        proj = h @ w_in[l]
        ua, va, q, k, v = jnp.split(proj, splits, axis=-1)
        y_a = gmlp_mixer(ua, va, sgu_ln_g[l], sgu_ln_b[l], w_spatial[l], b_spatial[l])
        y_b = dilated_attention(q.reshape(B, S, N_HEADS, HEAD_DIM),
                                k.reshape(B, S, N_HEADS, HEAD_DIM),
                                v.reshape(B, S, N_HEADS, HEAD_DIM))
        mix = jnp.concatenate([y_a, y_b], axis=-1) @ w_o[l]
        x = layer_norm(DEEPNORM_ALPHA * x + gate1 * mix, ln1_g[l], ln1_b[l])

        h = x * (1.0 + scale2) + shift2
        y = moe_ffn(h, w_router[l], b_router[l], w_exp1[l], b_exp1[l], w_exp2[l], b_exp2[l])
        x = layer_norm(DEEPNORM_ALPHA * x + gate2 * y, ln2_g[l], ln2_b[l])
    return x
```

```python
import numpy as np
import concourse.bass as bass
import concourse.mybir as mybir
from concourse.bass_utils import run_bass_kernel_spmd

F32 = mybir.dt.float32
F32R = mybir.dt.float32r
AF = mybir.ActivationFunctionType
ALU = mybir.AluOpType
AX = mybir.AxisListType

NCORE = 8
S = 8192
TL = S // NCORE
HALO = 2048
TE = TL + HALO
NE = 32
CAP = 384
EPS = 1e-5
BR = ((128, 1), (512, 4), (2048, 16))
NEG = -30000.0
ENGS = ("pe", "act", "dve", "pool", "sync")


class Prog:
    def __init__(self, sems, dma_sems):
        self.sems = sems
        self.dma_sems = dma_sems
        self.cnt = {k: 0 for k in sems}
        self.dcnt = [0] * len(dma_sems)
        self.drr = 0
        self.lastw = {}
        self.readers = {}
        self.seen = {e: {} for e in ENGS}
        self.ops = {e: [] for e in ENGS}

    def _deps(self, eng, reads, writes):
        deps = {}

        def add(tok):
            k, v = tok
            if deps.get(k, 0) < v:
                deps[k] = v
        for r in reads:
            if r in self.lastw:
                add(self.lastw[r])
        for w in writes:
            if w in self.lastw:
                add(self.lastw[w])
            for t in self.readers.get(w, ()):
                add(t)
        out = []
        for k, v in deps.items():
            if k == eng and eng == "pe":
                continue
            if self.seen[eng].get(k, 0) >= v:
                continue
            self.seen[eng][k] = v
            out.append((k, v))
        return out

    def _commit(self, tok, reads, writes):
        for r in reads:
            self.readers.setdefault(r, []).append(tok)
        for w in writes:
            self.lastw[w] = tok
            self.readers[w] = []

    def op(self, eng, fn, reads=(), writes=()):
        waits = self._deps(eng, reads, writes)
        self.cnt[eng] += 1
        self.ops[eng].append((waits, fn, (eng, 1)))
        self._commit((eng, self.cnt[eng]), reads, writes)

    def dma(self, q, fn, reads=(), writes=()):
        k = self.drr
        self.drr = (self.drr + 1) % len(self.dma_sems)
        waits = self._deps(q, reads, writes)
        key = ("d", k)
        if self.dcnt[k] > 0 and self.seen[q].get(key, 0) < self.dcnt[k]:
            self.seen[q][key] = self.dcnt[k]
            waits.append((key, self.dcnt[k]))
        self.dcnt[k] += 16
        self.ops[q].append((waits, fn, (key, 16)))
        self._commit((key, self.dcnt[k]), reads, writes)

    def fence(self):
        cur = [(k, v) for k, v in self.cnt.items() if v > 0]
        cur += [(("d", k), v) for k, v in enumerate(self.dcnt) if v > 0]
        for eng in ENGS:
            waits = []
            for k, v in cur:
                if k == eng:
                    continue
                if self.seen[eng].get(k, 0) < v:
                    self.seen[eng][k] = v
                    waits.append((k, v))
            if waits:
                self.ops[eng].append((waits, None, None))

    def _sem(self, key):
        if isinstance(key, tuple):
            return self.dma_sems[key[1]]
        return self.sems[key]

    def emit(self, eng, e):
        for waits, fn, inc in self.ops[eng]:
            for k, v in waits:
                e.wait_ge(self._sem(k), v)
            if fn is not None:
                fn(e).then_inc(self._sem(inc[0]), inc[1])


def build(D, stop_after=None):
    KC = D // 128
    G = H = D // 256
    DIN = 5 * D // 2
    NADA = 6 * D
    FW = min(512, D)
    NF = D // FW
    KG = min(4, KC)
    NKG = KC // KG
    BT = 256
    NB = TE // BT
    HB = HALO // BT
    C = CAP
    SC = C // 128
    NT = TL // 128
    OFF_U, OFF_V, OFF_Q, OFF_K, OFF_VA = 0, D // 2, D, D + D // 2, 2 * D
    ALPHA_DN = 2.0 ** 0.25
    ARN = 22 * 1024
    ARRN = 27 * 1024
    KX = min(2, KC)

    nc = bass.Bass("TRN2", target_bir_lowering=False)

    def din(name, shape):
        return nc.dram_tensor(name, list(shape), F32, kind="ExternalInput").ap()

    xe = din("xe", [TE, D])
    c_col = din("c_col", [128, KC])
    w_ada = din("w_ada", [D, NADA])
    b_ada = din("b_ada", [1, NADA])
    w_in = din("w_in", [D, DIN])
    sgu_g = din("sgu_g", [1, D // 2])
    sgu_b = din("sgu_b", [1, D // 2])
    wspT = din("wspT", [128, G * 128])
    tril = din("tril", [128, 128])
    bsp = din("bsp", [1, G * 128])
    w_o = din("w_o", [D, D])
    ln1_g = din("ln1_g", [1, D])
    ln1_b = din("ln1_b", [1, D])
    w_r = din("w_r", [128, KC * NE])
    b_r = din("b_r", [1, NE])
    if stop_after is None:
        w1 = din("w1", [NE * 2 * KC * 128, KC * 128])
        b1c = din("b1c", [128, NE * 2 * KC])
        w2 = din("w2", [NE * NF * NKG * 128, KG * FW])
        b2 = din("b2", [1, NE * D])
        ln2_g = din("ln2_g", [1, D])
        ln2_b = din("ln2_b", [1, D])
    abias = din("abias", [128, H * 6 * 128])
    kvalid = din("kvalid", [128, 32])
    ident_d = din("ident", [128, 128])
    iota_d = din("iota", [128, C])
    umat_d = din("umat", [128, 128])
    out_d = nc.dram_tensor("out", [TL, D], F32, kind="ExternalOutput").ap()

    ada_d = nc.dram_tensor("ada_d", [1, NADA], F32).ap()
    qkvT = nc.dram_tensor("qkvT", [3 * H * 128, TE], F32).ap()
    x1_d = nc.dram_tensor("x1_d", [TL, D], F32).ap()

    import contextlib
    es = contextlib.ExitStack()
    with es:
        def sb(name, shape, dt=F32):
            return es.enter_context(nc.sbuf_tensor(name, list(shape), dt))

        def sem(name):
            return es.enter_context(nc.semaphore(name))

        sems = {k: sem("s_" + k) for k in ("pe", "act", "dve", "pool")}
        dma_sems = [sem("d%d" % i) for i in range(12)]
        P = Prog(sems, dma_sems)
        ps = [es.enter_context(nc.psum_tensor("ps%d" % i, [128, 512], F32)) for i in range(8)]
        psn = [0]

        def nps():
            i = psn[0]
            psn[0] = (i + 1) % 8
            return ps[i], "ps%d" % i

        ident = sb("ident_sb", [128, 128])
        ones = sb("ones", [128, 128])
        umat = sb("umat_sb", [128, 128])
        iota = sb("iota_sb", [128, C])
        kval = sb("kval", [128, 32])
        adac = sb("adac", [128, 4 * KC])
        ccol = sb("ccol", [128, KC])
        csil = sb("csil", [128, KC])
        Gt = sb("Gt", [128, NT * NE])
        Mk = sb("Mk", [128, NT * NE])
        pos = sb("pos", [128, NT * NE])
        st6 = sb("st6", [128, 48])
        AR = sb("arena", [128, ARN])
        ARR = sb("arenar", [128, ARRN], F32R)
        P.dma("sync", lambda e: e.dma_start(out=ident[:], in_=ident_d), [], ["ident"])
        P.dma("sync", lambda e: e.dma_start(out=umat[:], in_=umat_d), [], ["umat"])
        P.dma("sync", lambda e: e.dma_start(out=iota[:], in_=iota_d), [], ["iota"])
        P.dma("sync", lambda e: e.dma_start(out=kval[:], in_=kvalid), [], ["kval"])
        P.op("dve", lambda e: e.memset(ones[:], 1.0), [], ["ones"])

        o = 0
        orr = 0

        def carve(n):
            nonlocal o
            a = AR[:, o:o + n]
            o += n
            assert o <= ARN, ("AR", o)
            return a

        def carver(n):
            nonlocal orr
            a = ARR[:, orr:orr + n]
            orr += n
            assert orr <= ARRN, ("ARR", orr)
            return a

        def bcast_row(dst, src_row_ap, res_r, res_w):
            P.dma("pool", lambda e: e.dma_start(out=dst, in_=src_row_ap.partition_broadcast(128)), res_r, res_w)

        def layer_norm(src, dst, gb, bb, rs, ws):
            for fc in range(NF):
                P.op("dve", lambda e, fc=fc: e.bn_stats(out=st6[:, fc * 6:(fc + 1) * 6], in_=src[:, fc * FW:(fc + 1) * FW]),
                     rs, ["st6"])
            P.op("dve", lambda e: e.bn_aggr(out=st6[:, 32:34], in_=st6[:, 0:NF * 6]), ["st6"], ["st6"])
            P.op("dve", lambda e: e.tensor_scalar(out=st6[:, 34:35], in0=st6[:, 33:34], scalar1=EPS, scalar2=None,
                                                 op0=ALU.add), ["st6"], ["st6"])
            P.op("act", lambda e: e.activation(out=st6[:, 35:36], in_=st6[:, 34:35], func=AF.Sqrt), ["st6"], ["st6"])
            P.op("dve", lambda e: e.reciprocal(out=st6[:, 36:37], in_=st6[:, 35:36]), ["st6"], ["st6"])
            P.op("dve", lambda e: e.tensor_scalar(out=dst, in0=src, scalar1=st6[:, 32:33], scalar2=st6[:, 36:37],
                                                 op0=ALU.subtract, op1=ALU.mult), list(rs) + ["st6"], ws)
            if gb is not None:
                P.op("dve", lambda e: e.tensor_tensor(out=dst, in0=dst, in1=gb, op=ALU.mult), list(ws) + ["lnp"], ws)
                P.op("dve", lambda e: e.tensor_tensor(out=dst, in0=dst, in1=bb, op=ALU.add), list(ws) + ["lnp"], ws)

        P.dma("sync", lambda e: e.dma_start(out=ccol[:], in_=c_col), [], ["ccol"])
        P.op("act", lambda e: e.activation(out=csil[:], in_=ccol[:], func=AF.Silu), ["ccol"], ["csil"])
        was = [carve(512) for _ in range(4)]
        brc = [carve(512) for _ in range(2)]
        arc = [carve(512) for _ in range(2)]
        rowb = carve(D)
        AW = 512
        for j in range(NADA // AW):
            pt, pn = nps()
            jb = j % 2
            P.dma("sync", lambda e, j=j, jb=jb: e.dma_start(out=brc[jb][0:1, :], in_=b_ada[0:1, j * AW:(j + 1) * AW]),
                  [], ["brc%d" % jb])
            for kc in range(KC):
                b = (j * KC + kc) % 4
                P.dma("sync", lambda e, b=b, kc=kc, j=j: e.dma_start(
                    out=was[b], in_=w_ada[kc * 128:(kc + 1) * 128, j * AW:(j + 1) * AW]), [], ["wa%d" % b])
                P.op("pe", lambda e, b=b, kc=kc, pt=pt: e.matmul(
                    pt[0:1, 0:AW], csil[:, kc:kc + 1], was[b], start=(kc == 0), stop=(kc == KC - 1)),
                    ["csil", "wa%d" % b], [pn])
            P.op("dve", lambda e, pt=pt, jb=jb: e.tensor_tensor(
                out=arc[jb][0:1, :], in0=pt[0:1, 0:AW], in1=brc[jb][0:1, :], op=ALU.add),
                [pn, "brc%d" % jb], ["arc%d" % jb])
            P.dma("sync", lambda e, j=j, jb=jb: e.dma_start(out=ada_d[0:1, j * AW:(j + 1) * AW], in_=arc[jb][0:1, :]),
                  ["arc%d" % jb], ["ada_d"])
        pt, pn = nps()
        for vi, v in enumerate((0, 1, 3, 4)):
            P.dma("sync", lambda e, v=v: e.dma_start(out=rowb[0:1, :], in_=ada_d[0:1, v * D:(v + 1) * D]),
                  ["ada_d"], ["rowb"])
            for kc in range(KC):
                P.op("pe", lambda e, pt=pt, vi=vi, kc=kc: e.matmul(
                    pt[:, vi * KC + kc:vi * KC + kc + 1], rowb[0:1, kc * 128:(kc + 1) * 128],
                    ones[0:1, 0:1], start=True, stop=True), ["rowb", "ones"], [pn])
        for vi in range(4):
            P.op("dve", lambda e, pt=pt, vi=vi: e.tensor_scalar(
                out=adac[:, vi * KC:(vi + 1) * KC], in0=pt[:, vi * KC:(vi + 1) * KC],
                scalar1=(1.0 if vi in (1, 3) else 0.0), scalar2=None, op0=ALU.add), [pn], ["adac"])
        sh1 = lambda kc: adac[:, 0 * KC + kc:0 * KC + kc + 1]
        sc1 = lambda kc: adac[:, 1 * KC + kc:1 * KC + kc + 1]
        sh2 = lambda kc: adac[:, 2 * KC + kc:2 * KC + kc + 1]
        sc2 = lambda kc: adac[:, 3 * KC + kc:3 * KC + kc + 1]
        P.fence()

        o = 0
        orr = 0
        YT = carver(KC * TL)
        hT = carver(KC * BT)
        wt = [carver(KC * 128) for _ in range(2)]
        wv = carver(KC * 128)
        xt = carve(D)
        UT = carve(G * BT)
        stmp = carve(128)
        sgb = carve(D // 2)
        sbb = carve(D // 2)
        bspb = carve(G * 128)
        WT = carve(G * 128)
        trl = carve(128)
        vg = carve(128)
        vln = carve(128)
        stg = [carve(BT) for _ in range(4)]
        bcast_row(sgb, sgu_g, [], ["sgb"])
        bcast_row(sbb, sgu_b, [], ["sbb"])
        bcast_row(bspb, bsp, [], ["bspb"])
        P.dma("sync", lambda e: e.dma_start(out=WT, in_=wspT), [], ["WT"])
        P.dma("sync", lambda e: e.dma_start(out=trl, in_=tril), [], ["trl"])
        for g in range(G):
            P.op("dve", lambda e, g=g: e.tensor_tensor(out=WT[:, g * 128:(g + 1) * 128], in0=WT[:, g * 128:(g + 1) * 128],
                                                    in1=trl, op=ALU.mult), ["WT", "trl"], ["WT"])
        w_in_v = w_in.rearrange("(kc p) n -> p kc n", p=128)
        nstg = [0]
        nwt = [0]
        for blk in range(NB):
            local = blk >= HB
            lb = blk - HB
            for tt in range(BT // 128):
                r0 = blk * BT + tt * 128
                P.dma("sync", lambda e, r0=r0: e.dma_start(out=xt, in_=xe[r0:r0 + 128, :]), [], ["xt"])
                for kc in range(KC):
                    if kc % 4 == 0:
                        pt, pn = nps()
                    P.op("pe", lambda e, pt=pt, kc=kc: e.transpose(
                        pt[:, (kc % 4) * 128:(kc % 4 + 1) * 128], xt[:, kc * 128:(kc + 1) * 128], ident[:]),
                        ["xt", "ident"], [pn])
                    P.op("act", lambda e, pt=pt, kc=kc, tt=tt: e.activation(
                        out=hT[:, kc * BT + tt * 128:kc * BT + (tt + 1) * 128],
                        in_=pt[:, (kc % 4) * 128:(kc % 4 + 1) * 128], func=AF.Identity,
                        scale=sc1(kc), bias=sh1(kc)), [pn, "adac"], ["hT"])
            chunks = []
            if local:
                chunks += [("u", g) for g in range(G)] + [("q", h) for h in range(H)]
            chunks += [("k", h) for h in range(H)] + [("v", h) for h in range(H)]
            for kind, idx in chunks:
                col0 = {"u": OFF_U, "q": OFF_Q, "k": OFF_K, "v": OFF_VA}[kind] + idx * 128
                b = nwt[0] % 2
                nwt[0] += 1
                P.dma("pool", lambda e, b=b, col0=col0: e.dma_start(
                    out=wt[b].rearrange("p (kc n) -> p kc n", kc=KC), in_=w_in_v[:, :, col0:col0 + 128]),
                    [], ["wt%d" % b])
                pt, pn = nps()
                for kc in range(KC):
                    P.op("pe", lambda e, pt=pt, b=b, kc=kc: e.matmul(
                        pt[:, 0:BT], wt[b][:, kc * 128:(kc + 1) * 128], hT[:, kc * BT:(kc + 1) * BT],
                        start=(kc == 0), stop=(kc == KC - 1)), ["wt%d" % b, "hT"], [pn])
                if kind == "u":
                    P.op("act", lambda e, pt=pt, idx=idx: e.activation(
                        out=UT[:, idx * BT:(idx + 1) * BT], in_=pt[:, 0:BT], func=AF.Gelu), [pn], ["UT"])
                else:
                    sgi = nstg[0] % 4
                    nstg[0] += 1
                    scl = (128.0 ** -0.5) if kind == "q" else 1.0
                    P.op("act", lambda e, pt=pt, sgi=sgi, scl=scl: e.activation(
                        out=stg[sgi], in_=pt[:, 0:BT], func=AF.Copy, scale=scl), [pn], ["stg%d" % sgi])
                    row0 = ({"q": 0, "k": H, "v": 2 * H}[kind] + idx) * 128
                    P.dma("sync", lambda e, sgi=sgi, row0=row0, blk=blk: e.dma_start(
                        out=qkvT[row0:row0 + 128, blk * BT:(blk + 1) * BT], in_=stg[sgi]),
                        ["stg%d" % sgi], ["qkvT"])
            if not local:
                continue
            for g in range(G):
                P.dma("pool", lambda e, g=g: e.dma_start(
                    out=wv.rearrange("p (kc n) -> p kc n", kc=KC),
                    in_=w_in_v[:, :, OFF_V + g * 128:OFF_V + (g + 1) * 128]), [], ["wv"])
                for tt in range(BT // 128):
                    pt, pn = nps()
                    for kc in range(KC):
                        P.op("pe", lambda e, pt=pt, kc=kc, tt=tt: e.matmul(
                            pt[:, 0:128], hT[:, kc * BT + tt * 128:kc * BT + (tt + 1) * 128],
                            wv[:, kc * 128:(kc + 1) * 128], start=(kc == 0), stop=(kc == KC - 1)),
                            ["wv", "hT"], [pn])
                    P.op("act", lambda e, pt=pt: e.activation(out=vg, in_=pt[:, 0:128], func=AF.Gelu), [pn], ["vg"])
                    P.op("dve", lambda e: e.bn_stats(out=st6[:, 0:6], in_=vg), ["vg"], ["st6"])
                    P.op("dve", lambda e: e.bn_aggr(out=st6[:, 32:34], in_=st6[:, 0:6]), ["st6"], ["st6"])
                    P.op("dve", lambda e: e.tensor_scalar(out=st6[:, 34:35], in0=st6[:, 33:34], scalar1=EPS,
                                                         scalar2=None, op0=ALU.add), ["st6"], ["st6"])
                    P.op("act", lambda e: e.activation(out=st6[:, 35:36], in_=st6[:, 34:35], func=AF.Sqrt),
                         ["st6"], ["st6"])
                    P.op("dve", lambda e: e.reciprocal(out=st6[:, 36:37], in_=st6[:, 35:36]), ["st6"], ["st6"])
                    P.op("dve", lambda e: e.tensor_scalar(out=vln, in0=vg, scalar1=st6[:, 32:33], scalar2=st6[:, 36:37],
                                                         op0=ALU.subtract, op1=ALU.mult), ["vg", "st6"], ["vln"])
                    P.op("dve", lambda e, g=g: e.tensor_tensor(out=vln, in0=vln, in1=sgb[:, g * 128:(g + 1) * 128],
                                                            op=ALU.mult), ["vln", "sgb"], ["vln"])
                    P.op("dve", lambda e, g=g: e.tensor_tensor(out=vln, in0=vln, in1=sbb[:, g * 128:(g + 1) * 128],
                                                            op=ALU.add), ["vln", "sbb"], ["vln"])
                    p2, p2n = nps()
                    P.op("pe", lambda e, p2=p2, g=g: e.matmul(p2[:, 0:128], vln, WT[:, g * 128:(g + 1) * 128],
                                                           start=True, stop=True), ["vln", "WT"], [p2n])
                    P.op("dve", lambda e, p2=p2, g=g: e.tensor_tensor(out=stmp, in0=p2[:, 0:128],
                                                                   in1=bspb[:, g * 128:(g + 1) * 128], op=ALU.add),
                         [p2n, "bspb"], ["stmp"])
                    tok0 = lb * BT + tt * 128
                    P.op("dve", lambda e, g=g, tt=tt, tok0=tok0: e.tensor_tensor(
                        out=YT[:, g * TL + tok0:g * TL + tok0 + 128], in0=stmp,
                        in1=UT[:, g * BT + tt * 128:g * BT + (tt + 1) * 128], op=ALU.mult), ["stmp", "UT"], ["YT"])
        P.fence()

        if stop_after != "1a":
            o = 0
            QTh = carve(TL)
            KTh = carve(TE)
            VTh = carve(TE)
            ABh = carve(6 * 128)
            NUM = carve(TL)
            DEN = carve(TL)
            RDN = carve(TL)
            Va = [carve(128) for _ in range(2)]
            PTt = [carve(128) for _ in range(2)]
            nva = [0]

            def sl(start, step, n):
                return slice(start, start + step * (n - 1) + 1, step)
            for h in range(H):
                P.dma("sync", lambda e, h=h: e.dma_start(out=QTh, in_=qkvT[h * 128:(h + 1) * 128, HALO:TE]), ["qkvT"], ["QTh"])
                P.dma("sync", lambda e, h=h: e.dma_start(out=KTh, in_=qkvT[(H + h) * 128:(H + h + 1) * 128, :]), ["qkvT"], ["KTh"])
                P.dma("sync", lambda e, h=h: e.dma_start(out=VTh, in_=qkvT[(2 * H + h) * 128:(2 * H + h + 1) * 128, :]), ["qkvT"], ["VTh"])
                P.dma("sync", lambda e, h=h: e.dma_start(out=ABh, in_=abias[:, h * 768:(h + 1) * 768]), [], ["ABh"])
                first = True
                for bi, (win, d) in enumerate(BR):
                    i_lo = HALO // d
                    n_loc = TL // d
                    for r in range(d):
                        for q0 in range(0, n_loc, 128):
                            nq = min(128, n_loc - q0)
                            i0 = i_lo + q0
                            qs = sl(r + d * q0, d, nq)
                            pnum, pnn = nps()
                            pden, pdn = nps()
                            for ch in range(2):
                                ik0 = i0 - 128 if ch == 0 else i0
                                nk = 128 if ch == 0 else nq
                                ks = sl(r + d * ik0, d, nk)
                                col = {1: 0, 4: 24, 16: 30}[d] + ik0 // 128
                                ab0 = (bi * 2 + ch) * 128
                                pS, psn_ = nps()
                                P.op("pe", lambda e, pS=pS, ks=ks, qs=qs, nk=nk, nq=nq: e.matmul(
                                    pS[0:nk, 0:nq], KTh[:, ks], QTh[:, qs], start=True, stop=False),
                                    ["KTh", "QTh"], [psn_])
                                P.op("pe", lambda e, pS=pS, nk=nk, nq=nq, ab0=ab0: e.matmul(
                                    pS[0:nk, 0:nq], ident[0:nk, 0:nk], ABh[0:nk, ab0:ab0 + nq], start=False, stop=True),
                                    ["ident", "ABh"], [psn_])
                                vb = nva[0] % 2
                                nva[0] += 1
                                P.op("act", lambda e, pS=pS, nk=nk, nq=nq, col=col, vb=vb: e.activation(
                                    out=PTt[vb][0:nk, 0:nq], in_=pS[0:nk, 0:nq], func=AF.Exp, bias=kval[0:nk, col:col + 1]),
                                    [psn_, "kval"], ["PT%d" % vb])
                                pV, pvn = nps()
                                P.op("pe", lambda e, pV=pV, ks=ks, nk=nk: e.transpose(pV[0:nk, 0:128], VTh[:, ks], ident[:]),
                                     ["VTh", "ident"], [pvn])
                                P.op("dve", lambda e, pV=pV, nk=nk, vb=vb: e.tensor_copy(out=Va[vb][0:nk, :], in_=pV[0:nk, 0:128]),
                                     [pvn], ["Va%d" % vb])
                                P.op("pe", lambda e, pnum=pnum, nk=nk, nq=nq, vb=vb, ch=ch: e.matmul(
                                    pnum[:, 0:nq], Va[vb][0:nk, :], PTt[vb][0:nk, 0:nq], start=(ch == 0), stop=(ch == 1)),
                                    ["Va%d" % vb, "PT%d" % vb], [pnn])
                                P.op("pe", lambda e, pden=pden, nk=nk, nq=nq, vb=vb, ch=ch: e.matmul(
                                    pden[0:1, 0:nq], ones[0:nk, 0:1], PTt[vb][0:nk, 0:nq], start=(ch == 0), stop=(ch == 1)),
                                    ["ones", "PT%d" % vb], [pdn])
                            if first:
                                P.op("dve", lambda e, pnum=pnum, qs=qs, nq=nq: e.tensor_copy(out=NUM[:, qs], in_=pnum[:, 0:nq]),
                                     [pnn], ["NUM"])
                                P.op("dve", lambda e, pden=pden, qs=qs, nq=nq: e.tensor_copy(out=DEN[0:1, qs], in_=pden[0:1, 0:nq]),
                                     [pdn], ["DEN"])
                            else:
                                P.op("dve", lambda e, pnum=pnum, qs=qs, nq=nq: e.tensor_tensor(
                                    out=NUM[:, qs], in0=NUM[:, qs], in1=pnum[:, 0:nq], op=ALU.add), [pnn, "NUM"], ["NUM"])
                                P.op("dve", lambda e, pden=pden, qs=qs, nq=nq: e.tensor_tensor(
                                    out=DEN[0:1, qs], in0=DEN[0:1, qs], in1=pden[0:1, 0:nq], op=ALU.add), [pdn, "DEN"], ["DEN"])
                    first = False
                P.op("dve", lambda e: e.reciprocal(out=RDN[0:1, :], in_=DEN[0:1, :]), ["DEN"], ["RDN"])
                for hf in range(TL // 512):
                    pb, pbn = nps()
                    P.op("pe", lambda e, pb=pb, hf=hf: e.matmul(pb[:, :], ones[0:1, 0:128], RDN[0:1, hf * 512:(hf + 1) * 512],
                                                             start=True, stop=True), ["ones", "RDN"], [pbn])
                    P.op("dve", lambda e, pb=pb, hf=hf, h=h: e.tensor_tensor(
                        out=YT[:, (G + h) * TL + hf * 512:(G + h) * TL + (hf + 1) * 512],
                        in0=NUM[:, hf * 512:(hf + 1) * 512], in1=pb[:, :], op=ALU.mult), [pbn, "NUM"], ["YT"])
            P.fence()

        if stop_after not in ("1a", "1b"):
            o = 0
            orr = KC * TL
            wo = carver(KC * FW)
            xt = carve(D)
            xs = carve(D)
            pre = carve(D)
            g1b = carve(D)
            l1g = carve(D)
            l1b = carve(D)
            h2t = carve(KC * 128)
            wr = carve(KC * NE)
            brr = carve(NE)
            lg = carve(NE)
            ex = carve(NE)
            m8 = carve(8)
            sm = carve(8)
            bcast_row(g1b, ada_d[0:1, 2 * D:3 * D], ["ada_d"], ["g1b"])
            bcast_row(l1g, ln1_g, [], ["lnp"])
            bcast_row(l1b, ln1_b, [], ["lnp"])
            P.dma("sync", lambda e: e.dma_start(out=wr, in_=w_r), [], ["wr"])
            P.dma("sync", lambda e: e.dma_start(out=brr[0:1, :], in_=b_r), [], ["brr"])
            w_o_v = w_o.rearrange("(kc p) n -> p kc n", p=128)
            for tt in range(NT):
                P.dma("sync", lambda e, tt=tt: e.dma_start(out=xt, in_=xe[HALO + tt * 128:HALO + (tt + 1) * 128, :]), [], ["xt"])
                P.op("act", lambda e: e.activation(out=xs, in_=xt, func=AF.Copy, scale=ALPHA_DN), ["xt"], ["xs"])
                for fc in range(NF):
                    P.dma("pool", lambda e, fc=fc: e.dma_start(out=wo.rearrange("p (kc n) -> p kc n", kc=KC),
                                                              in_=w_o_v[:, :, fc * FW:(fc + 1) * FW]), [], ["wo"])
                    pt, pn = nps()
                    for kc in range(KC):
                        P.op("pe", lambda e, pt=pt, kc=kc, tt=tt: e.matmul(
                            pt[:, 0:FW], YT[:, kc * TL + tt * 128:kc * TL + (tt + 1) * 128], wo[:, kc * FW:(kc + 1) * FW],
                            start=(kc == 0), stop=(kc == KC - 1)), ["YT", "wo"], [pn])
                    P.op("dve", lambda e, pt=pt, fc=fc: e.tensor_tensor(out=pre[:, fc * FW:(fc + 1) * FW], in0=pt[:, 0:FW],
                                                                      in1=g1b[:, fc * FW:(fc + 1) * FW], op=ALU.mult),
                         [pn, "g1b"], ["pre"])
                P.op("dve", lambda e: e.tensor_tensor(out=pre, in0=pre, in1=xs, op=ALU.add), ["pre", "xs"], ["pre"])
                layer_norm(pre, xs, l1g, l1b, ["pre"], ["xs"])
                P.dma("sync", lambda e, tt=tt: e.dma_start(out=x1_d[tt * 128:(tt + 1) * 128, :], in_=xs), ["xs"], ["x1_d"])
                for kc in range(KC):
                    if kc % 4 == 0:
                        pt, pn = nps()
                    P.op("pe", lambda e, pt=pt, kc=kc: e.transpose(pt[:, (kc % 4) * 128:(kc % 4 + 1) * 128],
                                                                xs[:, kc * 128:(kc + 1) * 128], ident[:]), ["xs", "ident"], [pn])
                    P.op("act", lambda e, pt=pt, kc=kc: e.activation(
                        out=h2t[:, kc * 128:(kc + 1) * 128], in_=pt[:, (kc % 4) * 128:(kc % 4 + 1) * 128],
                        func=AF.Identity, scale=sc2(kc), bias=sh2(kc)), [pn, "adac"], ["h2t"])
                pt, pn = nps()
                for kc in range(KC):
                    P.op("pe", lambda e, pt=pt, kc=kc: e.matmul(pt[:, 0:NE], h2t[:, kc * 128:(kc + 1) * 128],
                                                             wr[:, kc * NE:(kc + 1) * NE], start=(kc == 0), stop=False),
                         ["h2t", "wr"], [pn])
                P.op("pe", lambda e, pt=pt: e.matmul(pt[:, 0:NE], ones[0:1, 0:128], brr[0:1, :], start=False, stop=True),
                     ["ones", "brr"], [pn])
                mk = Mk[:, tt * NE:(tt + 1) * NE]
                P.op("dve", lambda e, pt=pt: e.tensor_copy(out=lg, in_=pt[:, 0:NE]), [pn], ["lg"])
                P.op("dve", lambda e: e.max(out=m8, in_=lg), ["lg"], ["m8"])
                P.op("dve", lambda e, mk=mk: e.tensor_scalar(out=mk, in0=lg, scalar1=m8[:, 3:4], scalar2=None, op0=ALU.is_ge),
                     ["lg", "m8"], ["Mk"])
                P.op("dve", lambda e: e.tensor_scalar(out=sm[:, 0:1], in0=m8[:, 0:1], scalar1=-1.0, scalar2=None, op0=ALU.mult),
                     ["m8"], ["sm"])
                P.op("act", lambda e: e.activation(out=ex, in_=lg, func=AF.Exp, bias=sm[:, 0:1]), ["lg", "sm"], ["ex"])
                P.op("dve", lambda e, mk=mk: e.tensor_tensor(out=ex, in0=ex, in1=mk, op=ALU.mult), ["ex", "Mk"], ["ex"])
                P.op("dve", lambda e: e.reduce_sum(out=sm[:, 1:2], in_=ex, axis=AX.X), ["ex"], ["sm"])
                P.op("dve", lambda e: e.reciprocal(out=sm[:, 2:3], in_=sm[:, 1:2]), ["sm"], ["sm"])
                P.op("dve", lambda e, tt=tt: e.tensor_scalar(out=Gt[:, tt * NE:(tt + 1) * NE], in0=ex, scalar1=sm[:, 2:3],
                                                            scalar2=None, op0=ALU.mult), ["ex", "sm"], ["Gt"])
            for tt in range(NT):
                pt, pn = nps()
                for t2 in range(tt):
                    P.op("pe", lambda e, pt=pt, t2=t2: e.matmul(pt[:, 0:NE], ones[:, :], Mk[:, t2 * NE:(t2 + 1) * NE],
                                                             start=(t2 == 0), stop=False), ["ones", "Mk"], [pn])
                P.op("pe", lambda e, pt=pt, tt=tt: e.matmul(pt[:, 0:NE], umat[:, :], Mk[:, tt * NE:(tt + 1) * NE],
                                                         start=(tt == 0), stop=True), ["umat", "Mk"], [pn])
                pp = pos[:, tt * NE:(tt + 1) * NE]
                P.op("dve", lambda e, pt=pt, pp=pp: e.tensor_scalar(out=pp, in0=pt[:, 0:NE], scalar1=1.0, scalar2=None,
                                                                  op0=ALU.add), [pn], ["pos"])
                P.op("dve", lambda e, pp=pp, tt=tt: e.tensor_tensor(out=pp, in0=pp, in1=Mk[:, tt * NE:(tt + 1) * NE],
                                                                  op=ALU.mult), ["pos", "Mk"], ["pos"])
                P.op("dve", lambda e, pp=pp: e.tensor_scalar(out=pp, in0=pp, scalar1=-1.0, scalar2=None, op0=ALU.add),
                     ["pos"], ["pos"])
            P.fence()

        if stop_after is None:
            o = 0
            orr = 0
            yacc = carve(NT * D)
            b2r = carve(D)
            b1s = carve(NE * 2 * KC)
            sg = [carve(C) for _ in range(2)]
            t1 = carve(C)
            t2 = carve(C)
            t3 = carve(C)
            xsl = carver(NT * KX * 128)
            XgT = carver(KC * C)
            actT = carver(KC * C)
            Zf = carver(SC * FW)
            assert NT * C == SC * TL
            Sel = carver(NT * C)
            SelGT = Sel
            o3 = o
            w1t = [carver(KC * 128) for _ in range(2)]
            w2t = [carver(KG * FW) for _ in range(2)]
            P.dma("sync", lambda e: e.dma_start(out=b1s, in_=b1c), [], ["b1s"])
            x1_v = x1_d.rearrange("(tt p) d -> p tt d", p=128)
            nw1 = [0]
            nw2 = [0]
            for ex_ in range(NE):
                P.dma("sync", lambda e, ex_=ex_: e.dma_start(out=b2r[0:1, :], in_=b2[0:1, ex_ * D:(ex_ + 1) * D]), [], ["b2r"])
                for tt in range(NT):
                    P.op("dve", lambda e, tt=tt, ex_=ex_: e.tensor_scalar(
                        out=Sel[:, tt * C:(tt + 1) * C], in0=iota[:, :], scalar1=pos[:, tt * NE + ex_:tt * NE + ex_ + 1],
                        scalar2=None, op0=ALU.is_equal), ["iota", "pos"], ["SelU"])
                for kg in range(KC // KX):
                    P.dma("pool", lambda e, kg=kg: e.dma_start(
                        out=xsl.rearrange("p (t n) -> p t n", t=NT), in_=x1_v[:, :, kg * KX * 128:(kg + 1) * KX * 128]),
                        ["x1_d"], ["xsl"])
                    for k4 in range(KX):
                        kc = kg * KX + k4
                        pt, pn = nps()
                        for tt in range(NT):
                            P.op("pe", lambda e, pt=pt, tt=tt, k4=k4: e.matmul(
                                pt[:, 0:C], xsl[:, tt * KX * 128 + k4 * 128:tt * KX * 128 + (k4 + 1) * 128],
                                Sel[:, tt * C:(tt + 1) * C], start=(tt == 0), stop=(tt == NT - 1)), ["xsl", "SelU"], [pn])
                        P.op("act", lambda e, pt=pt, kc=kc: e.activation(out=XgT[:, kc * C:(kc + 1) * C], in_=pt[:, 0:C],
                                                                       func=AF.Identity, scale=sc2(kc), bias=sh2(kc)),
                             [pn, "adac"], ["XgT"])
                for tt in range(NT):
                    sb_ = tt % 2
                    P.op("dve", lambda e, tt=tt, ex_=ex_, sb_=sb_: e.tensor_scalar(
                        out=sg[sb_], in0=iota[:, :], scalar1=pos[:, tt * NE + ex_:tt * NE + ex_ + 1],
                        scalar2=Gt[:, tt * NE + ex_:tt * NE + ex_ + 1], op0=ALU.is_equal, op1=ALU.mult),
                        ["iota", "pos", "Gt"], ["sg%d" % sb_])
                    pt, pn = nps()
                    for sc in range(SC):
                        P.op("pe", lambda e, pt=pt, sc=sc, sb_=sb_: e.transpose(
                            pt[:, sc * 128:(sc + 1) * 128], sg[sb_][:, sc * 128:(sc + 1) * 128], ident[:]),
                            ["sg%d" % sb_, "ident"], [pn])
                        P.op("act", lambda e, pt=pt, sc=sc, tt=tt: e.activation(
                            out=SelGT[:, sc * TL + tt * 128:sc * TL + (tt + 1) * 128], in_=pt[:, sc * 128:(sc + 1) * 128],
                            func=AF.Copy), [pn], ["SelU"])
                for j in range(KC):
                    pgl = []
                    for half in range(2):
                        b = nw1[0] % 2
                        nw1[0] += 1
                        row0 = (ex_ * 2 * KC + half * KC + j) * 128
                        P.dma("pool", lambda e, b=b, row0=row0: e.dma_start(out=w1t[b], in_=w1[row0:row0 + 128, :]),
                              [], ["w1t%d" % b])
                        pt, pn = nps()
                        for kc in range(KC):
                            P.op("pe", lambda e, pt=pt, b=b, kc=kc: e.matmul(
                                pt[:, 0:C], w1t[b][:, kc * 128:(kc + 1) * 128], XgT[:, kc * C:(kc + 1) * C],
                                start=(kc == 0), stop=(kc == KC - 1)), ["w1t%d" % b, "XgT"], [pn])
                        pgl.append((pt, pn))
                    (pg, pgn), (pl, pln) = pgl
                    cg = ex_ * 2 * KC + j
                    cl = ex_ * 2 * KC + KC + j
                    P.op("dve", lambda e, pg=pg, cg=cg: e.tensor_scalar(out=t1, in0=pg[:, 0:C], scalar1=b1s[:, cg:cg + 1],
                                                                       scalar2=7.0, op0=ALU.add, op1=ALU.min), [pgn, "b1s"], ["t1"])
                    P.op("dve", lambda e, pl=pl, cl=cl: e.tensor_scalar(out=t3, in0=pl[:, 0:C], scalar1=b1s[:, cl:cl + 1],
                                                                       scalar2=7.0, op0=ALU.add, op1=ALU.min), [pln, "b1s"], ["t3"])
                    P.op("act", lambda e: e.activation(out=t2, in_=t1, func=AF.Sigmoid, scale=1.702), ["t1"], ["t2"])
                    P.op("dve", lambda e: e.tensor_scalar(out=t3, in0=t3, scalar1=-7.0, scalar2=1.0, op0=ALU.max, op1=ALU.add),
                         ["t3"], ["t3"])
                    P.op("dve", lambda e: e.tensor_tensor(out=t1, in0=t1, in1=t2, op=ALU.mult), ["t1", "t2"], ["t1"])
                    P.op("dve", lambda e, j=j: e.tensor_tensor(out=actT[:, j * C:(j + 1) * C], in0=t1, in1=t3, op=ALU.mult),
                         ["t1", "t3"], ["actT"])
                for fc in range(NF):
                    pz = [nps() for _ in range(SC)]
                    for kg in range(NKG):
                        b = nw2[0] % 2
                        nw2[0] += 1
                        row0 = ((ex_ * NF + fc) * NKG + kg) * 128
                        P.dma("pool", lambda e, b=b, row0=row0: e.dma_start(out=w2t[b], in_=w2[row0:row0 + 128, :]),
                              [], ["w2t%d" % b])
                        for sc in range(SC):
                            for k4 in range(KG):
                                kc = kg * KG + k4
                                P.op("pe", lambda e, sc=sc, k4=k4, kc=kc, b=b, pz=pz: e.matmul(
                                    pz[sc][0][:, 0:FW], actT[:, kc * C + sc * 128:kc * C + (sc + 1) * 128],
                                    w2t[b][:, k4 * FW:(k4 + 1) * FW], start=(kc == 0), stop=False),
                                    ["actT", "w2t%d" % b], [pz[sc][1]])
                    for sc in range(SC):
                        P.op("pe", lambda e, sc=sc, fc=fc, pz=pz: e.matmul(
                            pz[sc][0][:, 0:FW], ones[0:1, 0:128], b2r[0:1, fc * FW:(fc + 1) * FW], start=False, stop=True),
                            ["ones", "b2r"], [pz[sc][1]])
                        P.op("act", lambda e, sc=sc, pz=pz: e.activation(out=Zf[:, sc * FW:(sc + 1) * FW], in_=pz[sc][0][:, 0:FW],
                                                                       func=AF.Copy), [pz[sc][1]], ["Zf"])
                    for tt in range(NT):
                        pt, pn = nps()
                        for sc in range(SC):
                            P.op("pe", lambda e, pt=pt, sc=sc, tt=tt: e.matmul(
                                pt[:, 0:FW], SelGT[:, sc * TL + tt * 128:sc * TL + (tt + 1) * 128], Zf[:, sc * FW:(sc + 1) * FW],
                                start=(sc == 0), stop=(sc == SC - 1)), ["SelU", "Zf"], [pn])
                        ya = yacc[:, tt * D + fc * FW:tt * D + (fc + 1) * FW]
                        if ex_ == 0:
                            P.op("dve", lambda e, pt=pt, ya=ya: e.tensor_copy(out=ya, in_=pt[:, 0:FW]), [pn], ["yacc"])
                        else:
                            P.op("dve", lambda e, pt=pt, ya=ya: e.tensor_tensor(out=ya, in0=ya, in1=pt[:, 0:FW], op=ALU.add),
                                 [pn, "yacc"], ["yacc"])
            P.fence()
            o = o3 - (D + NE * 2 * KC + 5 * C)
            assert o == NT * D, (o, NT * D)
            xt = carve(D)
            pc = [[carve(FW) for _ in range(2)] for _ in range(3)]
            npc = [0]

            def load_chunk(which, src_row, fc, rs):
                b = npc[0] % 2
                t = pc[which][b]
                nm = "pc%d_%d" % (which, b)
                bcast_row(t, src_row[0:1, fc * FW:(fc + 1) * FW], rs, [nm])
                return t, nm
            for tt in range(NT):
                ya = yacc[:, tt * D:(tt + 1) * D]
                P.dma("sync", lambda e, tt=tt: e.dma_start(out=xt, in_=x1_d[tt * 128:(tt + 1) * 128, :]), ["x1_d"], ["xt"])
                for fc in range(NF):
                    t, nm = load_chunk(0, ada_d[0:1, 5 * D:6 * D], fc, ["ada_d"])
                    npc[0] += 1
                    yc = ya[:, fc * FW:(fc + 1) * FW]
                    P.op("dve", lambda e, yc=yc, t=t: e.tensor_tensor(out=yc, in0=yc, in1=t, op=ALU.mult), ["yacc", nm], ["yacc"])
                P.op("act", lambda e: e.activation(out=xt, in_=xt, func=AF.Copy, scale=ALPHA_DN), ["xt"], ["xt"])
                P.op("dve", lambda e, ya=ya: e.tensor_tensor(out=ya, in0=ya, in1=xt, op=ALU.add), ["yacc", "xt"], ["yacc"])
                layer_norm(ya, ya, None, None, ["yacc"], ["yacc"])
                for fc in range(NF):
                    tg, ng = load_chunk(1, ln2_g, fc, [])
                    tb, nb_ = load_chunk(2, ln2_b, fc, [])
                    npc[0] += 1
                    yc = ya[:, fc * FW:(fc + 1) * FW]
                    P.op("dve", lambda e, yc=yc, tg=tg: e.tensor_tensor(out=yc, in0=yc, in1=tg, op=ALU.mult), ["yacc", ng], ["yacc"])
                    P.op("dve", lambda e, yc=yc, tb=tb: e.tensor_tensor(out=yc, in0=yc, in1=tb, op=ALU.add), ["yacc", nb_], ["yacc"])
                P.dma("sync", lambda e, tt=tt, ya=ya: e.dma_start(out=out_d[tt * 128:(tt + 1) * 128, :], in_=ya), ["yacc"], ["out"])

        if stop_after in ("1a", "1b"):
            ch0 = 0 if stop_after == "1a" else G
            for tt in range(NT):
                P.dma("sync", lambda e, tt=tt: e.dma_start(out=out_d[tt * 128:(tt + 1) * 128, 0:128],
                                                          in_=YT.bitcast(F32)[:, ch0 * TL + tt * 128:ch0 * TL + (tt + 1) * 128]),
                      ["YT"], ["out"])
        if stop_after == "1c":
            xt = carve(D)
            for tt in range(NT):
                P.dma("sync", lambda e, tt=tt: e.dma_start(out=xt, in_=x1_d[tt * 128:(tt + 1) * 128, :]), ["x1_d"], ["xt"])
                P.dma("sync", lambda e, tt=tt: e.dma_start(out=out_d[tt * 128:(tt + 1) * 128, :], in_=xt), ["xt"], ["out"])
        P.fence()
        with nc.Block() as block:
            @block.sync
            def _(e):
                P.emit("sync", e)

            @block.tensor
            def _(e):
                P.emit("pe", e)

            @block.scalar
            def _(e):
                P.emit("act", e)

            @block.vector
            def _(e):
                P.emit("dve", e)

            @block.gpsimd
            def _(e):
                P.emit("pool", e)
    return nc


def host_inputs(D, inp):
    KC = D // 128
    G = H = D // 256
    FW = min(512, D)
    NF = D // FW
    KG = min(4, KC)
    NKG = KC // KG
    f = lambda a: np.ascontiguousarray(np.asarray(a, dtype=np.float32))
    x = f(inp["x"])[0]
    c = f(inp["c"])[0]
    w_exp1 = np.asarray(inp["w_exp1"], dtype=np.float32)[0]
    w_exp2 = np.asarray(inp["w_exp2"], dtype=np.float32)[0]
    b_exp1 = f(inp["b_exp1"])[0]
    b_exp2 = f(inp["b_exp2"])[0]
    slopes = [2.0 ** (-8.0 * (h + 1) / H) for h in range(H)]
    ab = np.zeros((128, H, 3, 2, 128), np.float32)
    kk = np.arange(128)[:, None]
    qq = np.arange(128)[None, :]
    for h in range(H):
        for bi, (w, d) in enumerate(BR):
            ab[:, h, bi, 0, :] = np.where(kk >= qq, -slopes[h] * d * (qq + 128 - kk), NEG)
            ab[:, h, bi, 1, :] = np.where(qq - kk >= 0, -slopes[h] * d * (qq - kk), NEG)
    common = {
        "c_col": f(c.reshape(KC, 128).T),
        "w_ada": f(inp["w_ada"])[0], "b_ada": f(inp["b_ada"])[0].reshape(1, -1),
        "w_in": f(inp["w_in"])[0],
        "sgu_g": f(inp["sgu_ln_g"])[0].reshape(1, -1), "sgu_b": f(inp["sgu_ln_b"])[0].reshape(1, -1),
        "wspT": f(np.transpose(f(inp["w_spatial"])[0], (2, 0, 1)).reshape(128, G * 128)),
        "tril": f((np.arange(128)[:, None] <= np.arange(128)[None, :]).astype(np.float32)),
        "bsp": f(inp["b_spatial"])[0].reshape(1, -1),
        "w_o": f(inp["w_o"])[0],
        "ln1_g": f(inp["ln1_g"])[0].reshape(1, -1), "ln1_b": f(inp["ln1_b"])[0].reshape(1, -1),
        "w_r": f(f(inp["w_router"])[0].reshape(KC, 128, NE).transpose(1, 0, 2).reshape(128, KC * NE)),
        "b_r": f(inp["b_router"])[0].reshape(1, -1),
        "ln2_g": f(inp["ln2_g"])[0].reshape(1, -1), "ln2_b": f(inp["ln2_b"])[0].reshape(1, -1),
        "abias": ab.reshape(128, H * 6 * 128), "ident": np.eye(128, dtype=np.float32),
        "iota": f(np.broadcast_to(np.arange(CAP, dtype=np.float32), (128, CAP))),
        "umat": f((np.arange(128)[:, None] < np.arange(128)[None, :]).astype(np.float32)),
        "w1": f(w_exp1.reshape(NE, KC, 128, 2 * KC, 128).transpose(0, 3, 2, 1, 4).reshape(NE * 2 * KC * 128, KC * 128)),
        "b1c": f(b_exp1.reshape(NE, 2 * KC, 128).transpose(2, 0, 1).reshape(128, NE * 2 * KC)),
        "w2": f(w_exp2.reshape(NE, NKG, KG, 128, NF, FW).transpose(0, 4, 1, 3, 2, 5).reshape(NE * NF * NKG * 128, KG * FW)),
        "b2": f(b_exp2.reshape(1, NE * D)),
    }
    maps = []
    p = np.arange(128)
    for cidx in range(NCORE):
        m = dict(common)
        xe = np.zeros((TE, D), np.float32)
        lo = cidx * TL - HALO
        src0 = max(lo, 0)
        xe[src0 - lo:] = x[src0:cidx * TL + TL]
        m["xe"] = xe
        kv = np.zeros((128, 32), np.float32)
        first_valid = HALO - cidx * TL
        for ch in range(24):
            kv[:, ch] = np.where(ch * 128 + p >= first_valid, 0.0, NEG)
        for ch in range(6):
            kv[:, 24 + ch] = np.where((ch * 128 + p) * 4 >= first_valid, 0.0, NEG)
        for ch in range(2):
            kv[:, 30 + ch] = np.where((ch * 128 + p) * 16 >= first_valid, 0.0, NEG)
        m["kvalid"] = kv
        maps.append(m)
    return maps


_NC_CACHE = {}


def run(D, inp, stop_after=None):
    key = (D, stop_after)
    if key not in _NC_CACHE:
        _NC_CACHE[key] = build(D, stop_after=stop_after)
    nc = _NC_CACHE[key]
    maps = host_inputs(D, inp)
    names = set()
    for alloc in nc.allocations:
        if isinstance(alloc, mybir.MemoryLocationSet) and alloc.kind == "ExternalInput":
            names.add(alloc.memorylocations[0].name)
    maps = [{k: v for k, v in m.items() if k in names} for m in maps]
    return run_bass_kernel_spmd(nc, maps, core_ids=list(range(NCORE)))


def kernel(**inputs):
    D = int(np.asarray(inputs["x"]).shape[-1])
    res = run(D, inputs)
    out = np.concatenate([r["out"] for r in res.results], axis=0)
    return out.reshape(1, S, D).astype(np.float32)
```

```python
import numpy as np
import concourse.bass as bass
import concourse.mybir as mybir
from concourse.bass_utils import run_bass_kernel_spmd

F32 = mybir.dt.float32
F32R = mybir.dt.float32r
AF = mybir.ActivationFunctionType
ALU = mybir.AluOpType
AX = mybir.AxisListType

NCORE = 8
S = 8192
TL = S // NCORE
HALO = 2048
TE = TL + HALO
NE = 32
CAP = 384
EPS = 1e-5
BR = ((128, 1), (512, 4), (2048, 16))
NEG = -30000.0
ENGS = ("pe", "act", "dve", "pool", "sync")


class Prog:
    def __init__(self, sems, dma_sems):
        self.sems = sems
        self.dma_sems = dma_sems
        self.cnt = {k: 0 for k in sems}
        self.dcnt = [0] * len(dma_sems)
        self.drr = 0
        self.lastw = {}
        self.readers = {}
        self.seen = {e: {} for e in ENGS}
        self.ops = {e: [] for e in ENGS}

    def _deps(self, eng, reads, writes):
        deps = {}

        def add(tok):
            k, v = tok
            if deps.get(k, 0) < v:
                deps[k] = v
        for r in reads:
            if r in self.lastw:
                add(self.lastw[r])
        for w in writes:
            if w in self.lastw:
                add(self.lastw[w])
            for t in self.readers.get(w, ()):
                add(t)
        out = []
        for k, v in deps.items():
            if k == eng and eng == "pe":
                continue
            if self.seen[eng].get(k, 0) >= v:
                continue
            self.seen[eng][k] = v
            out.append((k, v))
        return out

    def _commit(self, tok, reads, writes):
        for r in reads:
            self.readers.setdefault(r, []).append(tok)
        for w in writes:
            self.lastw[w] = tok
            self.readers[w] = []

    def op(self, eng, fn, reads=(), writes=()):
        waits = self._deps(eng, reads, writes)
        self.cnt[eng] += 1
        self.ops[eng].append((waits, fn, (eng, 1)))
        self._commit((eng, self.cnt[eng]), reads, writes)

    def dma(self, q, fn, reads=(), writes=()):
        k = self.drr
        self.drr = (self.drr + 1) % len(self.dma_sems)
        waits = self._deps(q, reads, writes)
        key = ("d", k)
        if self.dcnt[k] > 0 and self.seen[q].get(key, 0) < self.dcnt[k]:
            self.seen[q][key] = self.dcnt[k]
            waits.append((key, self.dcnt[k]))
        self.dcnt[k] += 16
        self.ops[q].append((waits, fn, (key, 16)))
        self._commit((key, self.dcnt[k]), reads, writes)

    def fence(self):
        cur = [(k, v) for k, v in self.cnt.items() if v > 0]
        cur += [(("d", k), v) for k, v in enumerate(self.dcnt) if v > 0]
        for eng in ENGS:
            waits = []
            for k, v in cur:
                if k == eng:
                    continue
                if self.seen[eng].get(k, 0) < v:
                    self.seen[eng][k] = v
                    waits.append((k, v))
            if waits:
                self.ops[eng].append((waits, None, None))

    def _sem(self, key):
        if isinstance(key, tuple):
            return self.dma_sems[key[1]]
        return self.sems[key]

    def emit(self, eng, e):
        for waits, fn, inc in self.ops[eng]:
            for k, v in waits:
                e.wait_ge(self._sem(k), v)
            if fn is not None:
                fn(e).then_inc(self._sem(inc[0]), inc[1])


def build(D, stop_after=None):
    KC = D // 128
    G = H = D // 256
    DIN = 5 * D // 2
    NADA = 6 * D
    FW = min(512, D)
    NF = D // FW
    KG = min(4, KC)
    NKG = KC // KG
    BT = 256
    NB = TE // BT
    HB = HALO // BT
    C = CAP
    SC = C // 128
    NT = TL // 128
    OFF_U, OFF_V, OFF_Q, OFF_K, OFF_VA = 0, D // 2, D, D + D // 2, 2 * D
    ALPHA_DN = 2.0 ** 0.25
    ARN = 20 * 1024
    ARRN = 29 * 1024 + 512
    KX = min(2, KC)

    nc = bass.Bass("TRN2", target_bir_lowering=False)

    def din(name, shape):
        return nc.dram_tensor(name, list(shape), F32, kind="ExternalInput").ap()

    xe = din("xe", [TE, D])
    c_col = din("c_col", [128, KC])
    w_ada = din("w_ada", [D, NADA])
    b_ada = din("b_ada", [1, NADA])
    w_in = din("w_in", [D, DIN])
    sgu_g = din("sgu_g", [1, D // 2])
    sgu_b = din("sgu_b", [1, D // 2])
    wspT = din("wspT", [128, G * 128])
    tril = din("tril", [128, 128])
    bsp = din("bsp", [1, G * 128])
    w_o = din("w_o", [D, D])
    ln1_g = din("ln1_g", [1, D])
    ln1_b = din("ln1_b", [1, D])
    w_r = din("w_r", [128, KC * NE])
    b_r = din("b_r", [1, NE])
    if stop_after is None:
        w1 = nc.dram_tensor("w1", [NE * 2 * KC * 128, KC * 128], F32R, kind="ExternalInput").ap()
        b1c = din("b1c", [128, NE * 2 * KC])
        w2 = nc.dram_tensor("w2", [NE * NF * NKG * 128, KG * FW], F32R, kind="ExternalInput").ap()
        b2 = din("b2", [1, NE * D])
        ln2_g = din("ln2_g", [1, D])
        ln2_b = din("ln2_b", [1, D])
    abias = din("abias", [128, H * 6 * 128])
    kvalid = din("kvalid", [128, 32])
    ident_d = din("ident", [128, 128])
    iota_d = din("iota", [128, C])
    umat_d = din("umat", [128, 128])
    out_d = nc.dram_tensor("out", [TL, D], F32, kind="ExternalOutput").ap()

    ada_d = nc.dram_tensor("ada_d", [1, NADA], F32).ap()
    qkvT = nc.dram_tensor("qkvT", [3 * H * 128, TE], F32).ap()
    x1_d = nc.dram_tensor("x1_d", [TL, D], F32).ap()
    x1T_d = nc.dram_tensor("x1T_d", [KC * 128, TL], F32).ap()

    import contextlib
    es = contextlib.ExitStack()
    with es:
        def sb(name, shape, dt=F32):
            return es.enter_context(nc.sbuf_tensor(name, list(shape), dt))

        def sem(name):
            return es.enter_context(nc.semaphore(name))

        sems = {k: sem("s_" + k) for k in ("pe", "act", "dve", "pool")}
        dma_sems = [sem("d%d" % i) for i in range(12)]
        P = Prog(sems, dma_sems)
        ps = [es.enter_context(nc.psum_tensor("ps%d" % i, [128, 512], F32)) for i in range(8)]
        psn = [0]

        def nps():
            i = psn[0]
            psn[0] = (i + 1) % 8
            return ps[i], "ps%d" % i

        ident = sb("ident_sb", [128, 128])
        ones = sb("ones", [128, 128])
        umat = sb("umat_sb", [128, 128])
        iota = sb("iota_sb", [128, C])
        kval = sb("kval", [128, 32])
        adac = sb("adac", [128, 4 * KC])
        ccol = sb("ccol", [128, KC])
        csil = sb("csil", [128, KC])
        Gt = sb("Gt", [128, NT * NE])
        Mk = sb("Mk", [128, NT * NE])
        pos = sb("pos", [128, NT * NE])
        st6 = sb("st6", [128, 48])
        AR = sb("arena", [128, ARN])
        ARR = sb("arenar", [128, ARRN], F32R)
        P.dma("sync", lambda e: e.dma_start(out=ident[:], in_=ident_d), [], ["ident"])
        P.dma("sync", lambda e: e.dma_start(out=umat[:], in_=umat_d), [], ["umat"])
        P.dma("sync", lambda e: e.dma_start(out=iota[:], in_=iota_d), [], ["iota"])
        P.dma("sync", lambda e: e.dma_start(out=kval[:], in_=kvalid), [], ["kval"])
        P.op("dve", lambda e: e.memset(ones[:], 1.0), [], ["ones"])

        o = 0
        orr = 0

        def carve(n):
            nonlocal o
            a = AR[:, o:o + n]
            o += n
            assert o <= ARN, ("AR", o)
            return a

        def carver(n):
            nonlocal orr
            a = ARR[:, orr:orr + n]
            orr += n
            assert orr <= ARRN, ("ARR", orr)
            return a

        def bcast_row(dst, src_row_ap, res_r, res_w):
            P.dma("pool", lambda e: e.dma_start(out=dst, in_=src_row_ap.partition_broadcast(128)), res_r, res_w)

        def layer_norm(src, dst, gb, bb, rs, ws):
            for fc in range(NF):
                P.op("dve", lambda e, fc=fc: e.bn_stats(out=st6[:, fc * 6:(fc + 1) * 6], in_=src[:, fc * FW:(fc + 1) * FW]),
                     rs, ["st6"])
            P.op("dve", lambda e: e.bn_aggr(out=st6[:, 32:34], in_=st6[:, 0:NF * 6]), ["st6"], ["st6"])
            P.op("dve", lambda e: e.tensor_scalar(out=st6[:, 34:35], in0=st6[:, 33:34], scalar1=EPS, scalar2=None,
                                                 op0=ALU.add), ["st6"], ["st6"])
            P.op("act", lambda e: e.activation(out=st6[:, 35:36], in_=st6[:, 34:35], func=AF.Sqrt), ["st6"], ["st6"])
            P.op("dve", lambda e: e.reciprocal(out=st6[:, 36:37], in_=st6[:, 35:36]), ["st6"], ["st6"])
            P.op("dve", lambda e: e.tensor_scalar(out=dst, in0=src, scalar1=st6[:, 32:33], scalar2=st6[:, 36:37],
                                                 op0=ALU.subtract, op1=ALU.mult), list(rs) + ["st6"], ws)
            if gb is not None:
                P.op("dve", lambda e: e.tensor_tensor(out=dst, in0=dst, in1=gb, op=ALU.mult), list(ws) + ["lnp"], ws)
                P.op("dve", lambda e: e.tensor_tensor(out=dst, in0=dst, in1=bb, op=ALU.add), list(ws) + ["lnp"], ws)

        P.dma("sync", lambda e: e.dma_start(out=ccol[:], in_=c_col), [], ["ccol"])
        P.op("act", lambda e: e.activation(out=csil[:], in_=ccol[:], func=AF.Silu), ["ccol"], ["csil"])
        was = [carve(512) for _ in range(4)]
        brc = [carve(512) for _ in range(2)]
        arc = [carve(512) for _ in range(2)]
        rowb = carve(D)
        AW = 512
        for j in range(NADA // AW):
            pt, pn = nps()
            jb = j % 2
            P.dma("sync", lambda e, j=j, jb=jb: e.dma_start(out=brc[jb][0:1, :], in_=b_ada[0:1, j * AW:(j + 1) * AW]),
                  [], ["brc%d" % jb])
            for kc in range(KC):
                b = (j * KC + kc) % 4
                P.dma("sync", lambda e, b=b, kc=kc, j=j: e.dma_start(
                    out=was[b], in_=w_ada[kc * 128:(kc + 1) * 128, j * AW:(j + 1) * AW]), [], ["wa%d" % b])
                P.op("pe", lambda e, b=b, kc=kc, pt=pt: e.matmul(
                    pt[0:1, 0:AW], csil[:, kc:kc + 1], was[b], start=(kc == 0), stop=(kc == KC - 1)),
                    ["csil", "wa%d" % b], [pn])
            P.op("dve", lambda e, pt=pt, jb=jb: e.tensor_tensor(
                out=arc[jb][0:1, :], in0=pt[0:1, 0:AW], in1=brc[jb][0:1, :], op=ALU.add),
                [pn, "brc%d" % jb], ["arc%d" % jb])
            P.dma("sync", lambda e, j=j, jb=jb: e.dma_start(out=ada_d[0:1, j * AW:(j + 1) * AW], in_=arc[jb][0:1, :]),
                  ["arc%d" % jb], ["ada_d"])
        pt, pn = nps()
        for vi, v in enumerate((0, 1, 3, 4)):
            P.dma("sync", lambda e, v=v: e.dma_start(out=rowb[0:1, :], in_=ada_d[0:1, v * D:(v + 1) * D]),
                  ["ada_d"], ["rowb"])
            for kc in range(KC):
                P.op("pe", lambda e, pt=pt, vi=vi, kc=kc: e.matmul(
                    pt[:, vi * KC + kc:vi * KC + kc + 1], rowb[0:1, kc * 128:(kc + 1) * 128],
                    ones[0:1, 0:1], start=True, stop=True), ["rowb", "ones"], [pn])
        for vi in range(4):
            P.op("dve", lambda e, pt=pt, vi=vi: e.tensor_scalar(
                out=adac[:, vi * KC:(vi + 1) * KC], in0=pt[:, vi * KC:(vi + 1) * KC],
                scalar1=(1.0 if vi in (1, 3) else 0.0), scalar2=None, op0=ALU.add), [pn], ["adac"])
        sh1 = lambda kc: adac[:, 0 * KC + kc:0 * KC + kc + 1]
        sc1 = lambda kc: adac[:, 1 * KC + kc:1 * KC + kc + 1]
        sh2 = lambda kc: adac[:, 2 * KC + kc:2 * KC + kc + 1]
        sc2 = lambda kc: adac[:, 3 * KC + kc:3 * KC + kc + 1]
        P.fence()

        o = 0
        orr = 0
        YT = carver(KC * TL)
        hT = carver(KC * BT)
        wt = [carver(KC * 128) for _ in range(2)]
        wv = carver(KC * 128)
        xt = carve(D)
        UT = carve(G * BT)
        stmp = carve(128)
        sgb = carve(D // 2)
        sbb = carve(D // 2)
        bspb = carve(G * 128)
        WT = carve(G * 128)
        trl = carve(128)
        vg = carve(128)
        vln = carve(128)
        stg = [carve(BT) for _ in range(4)]
        bcast_row(sgb, sgu_g, [], ["sgb"])
        bcast_row(sbb, sgu_b, [], ["sbb"])
        bcast_row(bspb, bsp, [], ["bspb"])
        P.dma("sync", lambda e: e.dma_start(out=WT, in_=wspT), [], ["WT"])
        P.dma("sync", lambda e: e.dma_start(out=trl, in_=tril), [], ["trl"])
        for g in range(G):
            P.op("dve", lambda e, g=g: e.tensor_tensor(out=WT[:, g * 128:(g + 1) * 128], in0=WT[:, g * 128:(g + 1) * 128],
                                                    in1=trl, op=ALU.mult), ["WT", "trl"], ["WT"])
        w_in_v = w_in.bitcast(F32R).rearrange("(kc p) n -> p kc n", p=128)
        nstg = [0]
        nwt = [0]
        for blk in range(NB):
            local = blk >= HB
            lb = blk - HB
            for tt in range(BT // 128):
                r0 = blk * BT + tt * 128
                P.dma("sync", lambda e, r0=r0: e.dma_start(out=xt, in_=xe[r0:r0 + 128, :]), [], ["xt"])
                for kc in range(KC):
                    if kc % 4 == 0:
                        pt, pn = nps()
                    P.op("pe", lambda e, pt=pt, kc=kc: e.transpose(
                        pt[:, (kc % 4) * 128:(kc % 4 + 1) * 128], xt[:, kc * 128:(kc + 1) * 128], ident[:]),
                        ["xt", "ident"], [pn])
                    P.op("act", lambda e, pt=pt, kc=kc, tt=tt: e.activation(
                        out=hT[:, kc * BT + tt * 128:kc * BT + (tt + 1) * 128],
                        in_=pt[:, (kc % 4) * 128:(kc % 4 + 1) * 128], func=AF.Identity,
                        scale=sc1(kc), bias=sh1(kc)), [pn, "adac"], ["hT"])
            chunks = []
            if local:
                chunks += [("u", g) for g in range(G)] + [("q", h) for h in range(H)]
            chunks += [("k", h) for h in range(H)] + [("v", h) for h in range(H)]
            for kind, idx in chunks:
                col0 = {"u": OFF_U, "q": OFF_Q, "k": OFF_K, "v": OFF_VA}[kind] + idx * 128
                b = nwt[0] % 2
                nwt[0] += 1
                P.dma("pool", lambda e, b=b, col0=col0: e.dma_start(
                    out=wt[b].rearrange("p (kc n) -> p kc n", kc=KC), in_=w_in_v[:, :, col0:col0 + 128]),
                    [], ["wt%d" % b])
                pt, pn = nps()
                for kc in range(KC):
                    P.op("pe", lambda e, pt=pt, b=b, kc=kc: e.matmul(
                        pt[:, 0:BT], wt[b][:, kc * 128:(kc + 1) * 128], hT[:, kc * BT:(kc + 1) * BT],
                        start=(kc == 0), stop=(kc == KC - 1)), ["wt%d" % b, "hT"], [pn])
                if kind == "u":
                    P.op("act", lambda e, pt=pt, idx=idx: e.activation(
                        out=UT[:, idx * BT:(idx + 1) * BT], in_=pt[:, 0:BT], func=AF.Gelu), [pn], ["UT"])
                else:
                    sgi = nstg[0] % 4
                    nstg[0] += 1
                    scl = (128.0 ** -0.5) if kind == "q" else 1.0
                    P.op("act", lambda e, pt=pt, sgi=sgi, scl=scl: e.activation(
                        out=stg[sgi], in_=pt[:, 0:BT], func=AF.Copy, scale=scl), [pn], ["stg%d" % sgi])
                    row0 = ({"q": 0, "k": H, "v": 2 * H}[kind] + idx) * 128
                    P.dma("sync", lambda e, sgi=sgi, row0=row0, blk=blk: e.dma_start(
                        out=qkvT[row0:row0 + 128, blk * BT:(blk + 1) * BT], in_=stg[sgi]),
                        ["stg%d" % sgi], ["qkvT"])
            if not local:
                continue
            for g in range(G):
                P.dma("pool", lambda e, g=g: e.dma_start(
                    out=wv.rearrange("p (kc n) -> p kc n", kc=KC),
                    in_=w_in_v[:, :, OFF_V + g * 128:OFF_V + (g + 1) * 128]), [], ["wv"])
                for tt in range(BT // 128):
                    pt, pn = nps()
                    for kc in range(KC):
                        P.op("pe", lambda e, pt=pt, kc=kc, tt=tt: e.matmul(
                            pt[:, 0:128], hT[:, kc * BT + tt * 128:kc * BT + (tt + 1) * 128],
                            wv[:, kc * 128:(kc + 1) * 128], start=(kc == 0), stop=(kc == KC - 1)),
                            ["wv", "hT"], [pn])
                    P.op("act", lambda e, pt=pt: e.activation(out=vg, in_=pt[:, 0:128], func=AF.Gelu), [pn], ["vg"])
                    P.op("dve", lambda e: e.bn_stats(out=st6[:, 0:6], in_=vg), ["vg"], ["st6"])
                    P.op("dve", lambda e: e.bn_aggr(out=st6[:, 32:34], in_=st6[:, 0:6]), ["st6"], ["st6"])
                    P.op("dve", lambda e: e.tensor_scalar(out=st6[:, 34:35], in0=st6[:, 33:34], scalar1=EPS,
                                                         scalar2=None, op0=ALU.add), ["st6"], ["st6"])
                    P.op("act", lambda e: e.activation(out=st6[:, 35:36], in_=st6[:, 34:35], func=AF.Sqrt),
                         ["st6"], ["st6"])
                    P.op("dve", lambda e: e.reciprocal(out=st6[:, 36:37], in_=st6[:, 35:36]), ["st6"], ["st6"])
                    P.op("dve", lambda e: e.tensor_scalar(out=vln, in0=vg, scalar1=st6[:, 32:33], scalar2=st6[:, 36:37],
                                                         op0=ALU.subtract, op1=ALU.mult), ["vg", "st6"], ["vln"])
                    P.op("dve", lambda e, g=g: e.tensor_tensor(out=vln, in0=vln, in1=sgb[:, g * 128:(g + 1) * 128],
                                                            op=ALU.mult), ["vln", "sgb"], ["vln"])
                    P.op("dve", lambda e, g=g: e.tensor_tensor(out=vln, in0=vln, in1=sbb[:, g * 128:(g + 1) * 128],
                                                            op=ALU.add), ["vln", "sbb"], ["vln"])
                    p2, p2n = nps()
                    P.op("pe", lambda e, p2=p2, g=g: e.matmul(p2[:, 0:128], vln, WT[:, g * 128:(g + 1) * 128],
                                                           start=True, stop=True), ["vln", "WT"], [p2n])
                    P.op("dve", lambda e, p2=p2, g=g: e.tensor_tensor(out=stmp, in0=p2[:, 0:128],
                                                                   in1=bspb[:, g * 128:(g + 1) * 128], op=ALU.add),
                         [p2n, "bspb"], ["stmp"])
                    tok0 = lb * BT + tt * 128
                    P.op("dve", lambda e, g=g, tt=tt, tok0=tok0: e.tensor_tensor(
                        out=YT[:, g * TL + tok0:g * TL + tok0 + 128], in0=stmp,
                        in1=UT[:, g * BT + tt * 128:g * BT + (tt + 1) * 128], op=ALU.mult), ["stmp", "UT"], ["YT"])
        P.fence()

        if stop_after != "1a":
            o = 0
            QTh = carve(TL)
            KTh = carve(TE)
            VTh = carve(TE)
            ABh = carve(6 * 128)
            NUM = carve(TL)
            DEN = carve(TL)
            RDN = carve(TL)
            Va = [carve(128) for _ in range(2)]
            PTt = [carve(128) for _ in range(2)]
            nva = [0]

            def sl(start, step, n):
                return slice(start, start + step * (n - 1) + 1, step)
            for h in range(H):
                P.dma("sync", lambda e, h=h: e.dma_start(out=QTh, in_=qkvT[h * 128:(h + 1) * 128, HALO:TE]), ["qkvT"], ["QTh"])
                P.dma("sync", lambda e, h=h: e.dma_start(out=KTh, in_=qkvT[(H + h) * 128:(H + h + 1) * 128, :]), ["qkvT"], ["KTh"])
                P.dma("sync", lambda e, h=h: e.dma_start(out=VTh, in_=qkvT[(2 * H + h) * 128:(2 * H + h + 1) * 128, :]), ["qkvT"], ["VTh"])
                P.dma("sync", lambda e, h=h: e.dma_start(out=ABh, in_=abias[:, h * 768:(h + 1) * 768]), [], ["ABh"])
                first = True
                for bi, (win, d) in enumerate(BR):
                    i_lo = HALO // d
                    n_loc = TL // d
                    for r in range(d):
                        for q0 in range(0, n_loc, 128):
                            nq = min(128, n_loc - q0)
                            i0 = i_lo + q0
                            qs = sl(r + d * q0, d, nq)
                            pnum, pnn = nps()
                            pden, pdn = nps()
                            for ch in range(2):
                                ik0 = i0 - 128 if ch == 0 else i0
                                nk = 128 if ch == 0 else nq
                                ks = sl(r + d * ik0, d, nk)
                                col = {1: 0, 4: 24, 16: 30}[d] + ik0 // 128
                                ab0 = (bi * 2 + ch) * 128
                                pS, psn_ = nps()
                                P.op("pe", lambda e, pS=pS, ks=ks, qs=qs, nk=nk, nq=nq: e.matmul(
                                    pS[0:nk, 0:nq], KTh[:, ks], QTh[:, qs], start=True, stop=False),
                                    ["KTh", "QTh"], [psn_])
                                P.op("pe", lambda e, pS=pS, nk=nk, nq=nq, ab0=ab0: e.matmul(
                                    pS[0:nk, 0:nq], ident[0:nk, 0:nk], ABh[0:nk, ab0:ab0 + nq], start=False, stop=True),
                                    ["ident", "ABh"], [psn_])
                                vb = nva[0] % 2
                                nva[0] += 1
                                P.op("act", lambda e, pS=pS, nk=nk, nq=nq, col=col, vb=vb: e.activation(
                                    out=PTt[vb][0:nk, 0:nq], in_=pS[0:nk, 0:nq], func=AF.Exp, bias=kval[0:nk, col:col + 1]),
                                    [psn_, "kval"], ["PT%d" % vb])
                                pV, pvn = nps()
                                P.op("pe", lambda e, pV=pV, ks=ks, nk=nk: e.transpose(pV[0:nk, 0:128], VTh[:, ks], ident[:]),
                                     ["VTh", "ident"], [pvn])
                                P.op("dve", lambda e, pV=pV, nk=nk, vb=vb: e.tensor_copy(out=Va[vb][0:nk, :], in_=pV[0:nk, 0:128]),
                                     [pvn], ["Va%d" % vb])
                                P.op("pe", lambda e, pnum=pnum, nk=nk, nq=nq, vb=vb, ch=ch: e.matmul(
                                    pnum[:, 0:nq], Va[vb][0:nk, :], PTt[vb][0:nk, 0:nq], start=(ch == 0), stop=(ch == 1)),
                                    ["Va%d" % vb, "PT%d" % vb], [pnn])
                                P.op("pe", lambda e, pden=pden, nk=nk, nq=nq, vb=vb, ch=ch: e.matmul(
                                    pden[0:1, 0:nq], ones[0:nk, 0:1], PTt[vb][0:nk, 0:nq], start=(ch == 0), stop=(ch == 1)),
                                    ["ones", "PT%d" % vb], [pdn])
                            if first:
                                P.op("dve", lambda e, pnum=pnum, qs=qs, nq=nq: e.tensor_copy(out=NUM[:, qs], in_=pnum[:, 0:nq]),
                                     [pnn], ["NUM"])
                                P.op("dve", lambda e, pden=pden, qs=qs, nq=nq: e.tensor_copy(out=DEN[0:1, qs], in_=pden[0:1, 0:nq]),
                                     [pdn], ["DEN"])
                            else:
                                P.op("dve", lambda e, pnum=pnum, qs=qs, nq=nq: e.tensor_tensor(
                                    out=NUM[:, qs], in0=NUM[:, qs], in1=pnum[:, 0:nq], op=ALU.add), [pnn, "NUM"], ["NUM"])
                                P.op("dve", lambda e, pden=pden, qs=qs, nq=nq: e.tensor_tensor(
                                    out=DEN[0:1, qs], in0=DEN[0:1, qs], in1=pden[0:1, 0:nq], op=ALU.add), [pdn, "DEN"], ["DEN"])
                    first = False
                P.op("dve", lambda e: e.reciprocal(out=RDN[0:1, :], in_=DEN[0:1, :]), ["DEN"], ["RDN"])
                for hf in range(TL // 512):
                    pb, pbn = nps()
                    P.op("pe", lambda e, pb=pb, hf=hf: e.matmul(pb[:, :], ones[0:1, 0:128], RDN[0:1, hf * 512:(hf + 1) * 512],
                                                             start=True, stop=True), ["ones", "RDN"], [pbn])
                    P.op("dve", lambda e, pb=pb, hf=hf, h=h: e.tensor_tensor(
                        out=YT[:, (G + h) * TL + hf * 512:(G + h) * TL + (hf + 1) * 512],
                        in0=NUM[:, hf * 512:(hf + 1) * 512], in1=pb[:, :], op=ALU.mult), [pbn, "NUM"], ["YT"])
            P.fence()

        if stop_after not in ("1a", "1b"):
            o = 0
            orr = KC * TL
            wo = carver(KC * FW)
            xt = carve(D)
            xs = carve(D)
            pre = carve(D)
            g1b = carve(D)
            l1g = carve(D)
            l1b = carve(D)
            h2t = carve(KC * 128)
            wr = carve(KC * NE)
            brr = carve(NE)
            lg = carve(NE)
            ex = carve(NE)
            m8 = carve(8)
            sm = carve(8)
            bcast_row(g1b, ada_d[0:1, 2 * D:3 * D], ["ada_d"], ["g1b"])
            bcast_row(l1g, ln1_g, [], ["lnp"])
            bcast_row(l1b, ln1_b, [], ["lnp"])
            P.dma("sync", lambda e: e.dma_start(out=wr, in_=w_r), [], ["wr"])
            P.dma("sync", lambda e: e.dma_start(out=brr[0:1, :], in_=b_r), [], ["brr"])
            w_o_v = w_o.bitcast(F32R).rearrange("(kc p) n -> p kc n", p=128)
            for tt in range(NT):
                P.dma("sync", lambda e, tt=tt: e.dma_start(out=xt, in_=xe[HALO + tt * 128:HALO + (tt + 1) * 128, :]), [], ["xt"])
                P.op("act", lambda e: e.activation(out=xs, in_=xt, func=AF.Copy, scale=ALPHA_DN), ["xt"], ["xs"])
                for fc in range(NF):
                    P.dma("pool", lambda e, fc=fc: e.dma_start(out=wo.rearrange("p (kc n) -> p kc n", kc=KC),
                                                              in_=w_o_v[:, :, fc * FW:(fc + 1) * FW]), [], ["wo"])
                    pt, pn = nps()
                    for kc in range(KC):
                        P.op("pe", lambda e, pt=pt, kc=kc, tt=tt: e.matmul(
                            pt[:, 0:FW], YT[:, kc * TL + tt * 128:kc * TL + (tt + 1) * 128], wo[:, kc * FW:(kc + 1) * FW],
                            start=(kc == 0), stop=(kc == KC - 1)), ["YT", "wo"], [pn])
                    P.op("dve", lambda e, pt=pt, fc=fc: e.tensor_tensor(out=pre[:, fc * FW:(fc + 1) * FW], in0=pt[:, 0:FW],
                                                                      in1=g1b[:, fc * FW:(fc + 1) * FW], op=ALU.mult),
                         [pn, "g1b"], ["pre"])
                P.op("dve", lambda e: e.tensor_tensor(out=pre, in0=pre, in1=xs, op=ALU.add), ["pre", "xs"], ["pre"])
                layer_norm(pre, xs, l1g, l1b, ["pre"], ["xs"])
                P.dma("sync", lambda e, tt=tt: e.dma_start(out=x1_d[tt * 128:(tt + 1) * 128, :], in_=xs), ["xs"], ["x1_d"])
                P.dma("sync", lambda e, tt=tt: e.dma_start(
                    out=x1T_d.rearrange("(kc p) t -> p kc t", p=128)[:, :, tt * 128:(tt + 1) * 128],
                    in_=xs.rearrange("p (kc n) -> p kc n", kc=KC)), ["xs"], ["x1T_d"])
                for kc in range(KC):
                    if kc % 4 == 0:
                        pt, pn = nps()
                    P.op("pe", lambda e, pt=pt, kc=kc: e.transpose(pt[:, (kc % 4) * 128:(kc % 4 + 1) * 128],
                                                                xs[:, kc * 128:(kc + 1) * 128], ident[:]), ["xs", "ident"], [pn])
                    P.op("act", lambda e, pt=pt, kc=kc: e.activation(
                        out=h2t[:, kc * 128:(kc + 1) * 128], in_=pt[:, (kc % 4) * 128:(kc % 4 + 1) * 128],
                        func=AF.Identity, scale=sc2(kc), bias=sh2(kc)), [pn, "adac"], ["h2t"])
                pt, pn = nps()
                for kc in range(KC):
                    P.op("pe", lambda e, pt=pt, kc=kc: e.matmul(pt[:, 0:NE], h2t[:, kc * 128:(kc + 1) * 128],
                                                             wr[:, kc * NE:(kc + 1) * NE], start=(kc == 0), stop=False),
                         ["h2t", "wr"], [pn])
                P.op("pe", lambda e, pt=pt: e.matmul(pt[:, 0:NE], ones[0:1, 0:128], brr[0:1, :], start=False, stop=True),
                     ["ones", "brr"], [pn])
                mk = Mk[:, tt * NE:(tt + 1) * NE]
                P.op("dve", lambda e, pt=pt: e.tensor_copy(out=lg, in_=pt[:, 0:NE]), [pn], ["lg"])
                P.op("dve", lambda e: e.max(out=m8, in_=lg), ["lg"], ["m8"])
                P.op("dve", lambda e, mk=mk: e.tensor_scalar(out=mk, in0=lg, scalar1=m8[:, 3:4], scalar2=None, op0=ALU.is_ge),
                     ["lg", "m8"], ["Mk"])
                P.op("dve", lambda e: e.tensor_scalar(out=sm[:, 0:1], in0=m8[:, 0:1], scalar1=-1.0, scalar2=None, op0=ALU.mult),
                     ["m8"], ["sm"])
                P.op("act", lambda e: e.activation(out=ex, in_=lg, func=AF.Exp, bias=sm[:, 0:1]), ["lg", "sm"], ["ex"])
                P.op("dve", lambda e, mk=mk: e.tensor_tensor(out=ex, in0=ex, in1=mk, op=ALU.mult), ["ex", "Mk"], ["ex"])
                P.op("dve", lambda e: e.reduce_sum(out=sm[:, 1:2], in_=ex, axis=AX.X), ["ex"], ["sm"])
                P.op("dve", lambda e: e.reciprocal(out=sm[:, 2:3], in_=sm[:, 1:2]), ["sm"], ["sm"])
                P.op("dve", lambda e, tt=tt: e.tensor_scalar(out=Gt[:, tt * NE:(tt + 1) * NE], in0=ex, scalar1=sm[:, 2:3],
                                                            scalar2=None, op0=ALU.mult), ["ex", "sm"], ["Gt"])
            for tt in range(NT):
                pt, pn = nps()
                for t2 in range(tt):
                    P.op("pe", lambda e, pt=pt, t2=t2: e.matmul(pt[:, 0:NE], ones[:, :], Mk[:, t2 * NE:(t2 + 1) * NE],
                                                             start=(t2 == 0), stop=False), ["ones", "Mk"], [pn])
                P.op("pe", lambda e, pt=pt, tt=tt: e.matmul(pt[:, 0:NE], umat[:, :], Mk[:, tt * NE:(tt + 1) * NE],
                                                         start=(tt == 0), stop=True), ["umat", "Mk"], [pn])
                pp = pos[:, tt * NE:(tt + 1) * NE]
                P.op("dve", lambda e, pt=pt, pp=pp: e.tensor_scalar(out=pp, in0=pt[:, 0:NE], scalar1=1.0, scalar2=None,
                                                                  op0=ALU.add), [pn], ["pos"])
                P.op("dve", lambda e, pp=pp, tt=tt: e.tensor_tensor(out=pp, in0=pp, in1=Mk[:, tt * NE:(tt + 1) * NE],
                                                                  op=ALU.mult), ["pos", "Mk"], ["pos"])
                P.op("dve", lambda e, pp=pp: e.tensor_scalar(out=pp, in0=pp, scalar1=-1.0, scalar2=None, op0=ALU.add),
                     ["pos"], ["pos"])
            P.fence()

        if stop_after is None:
            o = 0
            orr = 0
            yacc = carve(NT * D)
            b2c = [carve(FW) for _ in range(2)]
            b1s = carve(NE * 2 * KC)
            sg = [carve(C) for _ in range(2)]
            t1 = carve(C)
            t2 = carve(C)
            t3 = carve(C)
            xsl = [carver(NT * 128) for _ in range(2)]
            XgT = carver(KC * C)
            actT = carver(KC * C)
            Zf = carver(SC * FW)
            assert NT * C == SC * TL
            Sel = carver(NT * C)
            SelGT = Sel
            NW1 = 3
            w1t = [carver(KC * 128) for _ in range(NW1)]
            w2t = [carver(KG * FW) for _ in range(2)]
            P.dma("sync", lambda e: e.dma_start(out=b1s, in_=b1c), [], ["b1s"])
            nw1 = [0]
            nw2 = [0]
            nb2 = [0]

            def issue_w1(ex_, t):
                j, half = t
                b = nw1[0] % NW1
                nw1[0] += 1
                row0 = (ex_ * 2 * KC + half * KC + j) * 128
                P.dma("pool", lambda e, b=b, row0=row0: e.dma_start(out=w1t[b], in_=w1[row0:row0 + 128, :]),
                      [], ["w1t%d" % b])
                return b

            def issue_w2(ex_, t):
                fc, kg = t
                b = nw2[0] % 2
                nw2[0] += 1
                row0 = ((ex_ * NF + fc) * NKG + kg) * 128
                P.dma("pool", lambda e, b=b, row0=row0: e.dma_start(out=w2t[b], in_=w2[row0:row0 + 128, :]),
                      [], ["w2t%d" % b])
                return b

            def issue_x(kc):
                b = kc % 2
                P.dma("pool", lambda e, b=b, kc=kc: e.dma_start(out=xsl[b], in_=x1T_d.bitcast(F32R)[kc * 128:(kc + 1) * 128, :]),
                      ["x1T_d"], ["xsl%d" % b])
                return b
            w1_tiles = [(j, half) for j in range(KC) for half in range(2)]
            w2_tiles = [(fc, kg) for fc in range(NF) for kg in range(NKG)]
            for ex_ in range(NE):
                for tt in range(NT):
                    P.op("dve", lambda e, tt=tt, ex_=ex_: e.tensor_scalar(
                        out=Sel[:, tt * C:(tt + 1) * C], in0=iota[:, :], scalar1=pos[:, tt * NE + ex_:tt * NE + ex_ + 1],
                        scalar2=None, op0=ALU.is_equal), ["iota", "pos"], ["SelU"])
                xq = [issue_x(kc) for kc in range(min(2, KC))]
                w1q = [issue_w1(ex_, t) for t in w1_tiles[:NW1]]
                for kc in range(KC):
                    b = xq[kc]
                    pt, pn = nps()
                    for tt in range(NT):
                        P.op("pe", lambda e, pt=pt, tt=tt, b=b: e.matmul(
                            pt[:, 0:C], xsl[b][:, tt * 128:(tt + 1) * 128],
                            Sel[:, tt * C:(tt + 1) * C], start=(tt == 0), stop=(tt == NT - 1)), ["xsl%d" % b, "SelU"], [pn])
                    if kc + 2 < KC:
                        xq.append(issue_x(kc + 2))
                    P.op("act", lambda e, pt=pt, kc=kc: e.activation(out=XgT[:, kc * C:(kc + 1) * C], in_=pt[:, 0:C],
                                                                   func=AF.Identity, scale=sc2(kc), bias=sh2(kc)),
                         [pn, "adac"], ["XgT"])
                w2q = [issue_w2(ex_, t) for t in w2_tiles[:2]]
                for tt in range(NT):
                    sb_ = tt % 2
                    P.op("dve", lambda e, tt=tt, ex_=ex_, sb_=sb_: e.tensor_scalar(
                        out=sg[sb_], in0=iota[:, :], scalar1=pos[:, tt * NE + ex_:tt * NE + ex_ + 1],
                        scalar2=Gt[:, tt * NE + ex_:tt * NE + ex_ + 1], op0=ALU.is_equal, op1=ALU.mult),
                        ["iota", "pos", "Gt"], ["sg%d" % sb_])
                    pt, pn = nps()
                    for sc in range(SC):
                        P.op("pe", lambda e, pt=pt, sc=sc, sb_=sb_: e.transpose(
                            pt[:, sc * 128:(sc + 1) * 128], sg[sb_][:, sc * 128:(sc + 1) * 128], ident[:]),
                            ["sg%d" % sb_, "ident"], [pn])
                        P.op("act", lambda e, pt=pt, sc=sc, tt=tt: e.activation(
                            out=SelGT[:, sc * TL + tt * 128:sc * TL + (tt + 1) * 128], in_=pt[:, sc * 128:(sc + 1) * 128],
                            func=AF.Copy), [pn], ["SelU"])
                pgl = []
                for ti, (j, half) in enumerate(w1_tiles):
                    b = w1q[ti]
                    pt, pn = nps()
                    for kc in range(KC):
                        P.op("pe", lambda e, pt=pt, b=b, kc=kc: e.matmul(
                            pt[:, 0:C], w1t[b][:, kc * 128:(kc + 1) * 128], XgT[:, kc * C:(kc + 1) * C],
                            start=(kc == 0), stop=(kc == KC - 1)), ["w1t%d" % b, "XgT"], [pn])
                    if ti + NW1 < len(w1_tiles):
                        w1q.append(issue_w1(ex_, w1_tiles[ti + NW1]))
                    pgl.append((pt, pn))
                    if half == 0:
                        continue
                    (pg, pgn), (pl, pln) = pgl
                    pgl = []
                    cg = ex_ * 2 * KC + j
                    cl = ex_ * 2 * KC + KC + j
                    P.op("dve", lambda e, pg=pg, cg=cg: e.tensor_scalar(out=t1, in0=pg[:, 0:C], scalar1=b1s[:, cg:cg + 1],
                                                                       scalar2=7.0, op0=ALU.add, op1=ALU.min), [pgn, "b1s"], ["t1"])
                    P.op("dve", lambda e, pl=pl, cl=cl: e.tensor_scalar(out=t3, in0=pl[:, 0:C], scalar1=b1s[:, cl:cl + 1],
                                                                       scalar2=7.0, op0=ALU.add, op1=ALU.min), [pln, "b1s"], ["t3"])
                    P.op("act", lambda e: e.activation(out=t2, in_=t1, func=AF.Sigmoid, scale=1.702), ["t1"], ["t2"])
                    P.op("dve", lambda e: e.tensor_scalar(out=t3, in0=t3, scalar1=-7.0, scalar2=1.0, op0=ALU.max, op1=ALU.add),
                         ["t3"], ["t3"])
                    P.op("dve", lambda e: e.tensor_tensor(out=t1, in0=t1, in1=t2, op=ALU.mult), ["t1", "t2"], ["t1"])
                    P.op("dve", lambda e, j=j: e.tensor_tensor(out=actT[:, j * C:(j + 1) * C], in0=t1, in1=t3, op=ALU.mult),
                         ["t1", "t3"], ["actT"])
                for fc in range(NF):
                    bb = nb2[0] % 2
                    nb2[0] += 1
                    P.dma("sync", lambda e, ex_=ex_, fc=fc, bb=bb: e.dma_start(
                        out=b2c[bb][0:1, :], in_=b2[0:1, ex_ * D + fc * FW:ex_ * D + (fc + 1) * FW]), [], ["b2c%d" % bb])
                    pz = [nps() for _ in range(SC)]
                    for kg in range(NKG):
                        ti = fc * NKG + kg
                        b = w2q[ti]
                        for sc in range(SC):
                            for k4 in range(KG):
                                kc = kg * KG + k4
                                P.op("pe", lambda e, sc=sc, k4=k4, kc=kc, b=b, pz=pz: e.matmul(
                                    pz[sc][0][:, 0:FW], actT[:, kc * C + sc * 128:kc * C + (sc + 1) * 128],
                                    w2t[b][:, k4 * FW:(k4 + 1) * FW], start=(kc == 0), stop=False),
                                    ["actT", "w2t%d" % b], [pz[sc][1]])
                        if ti + 2 < len(w2_tiles):
                            w2q.append(issue_w2(ex_, w2_tiles[ti + 2]))
                    for sc in range(SC):
                        P.op("pe", lambda e, sc=sc, bb=bb, pz=pz: e.matmul(
                            pz[sc][0][:, 0:FW], ones[0:1, 0:128], b2c[bb][0:1, :], start=False, stop=True),
                            ["ones", "b2c%d" % bb], [pz[sc][1]])
                        P.op("act", lambda e, sc=sc, pz=pz: e.activation(out=Zf[:, sc * FW:(sc + 1) * FW], in_=pz[sc][0][:, 0:FW],
                                                                       func=AF.Copy), [pz[sc][1]], ["Zf"])
                    for tt in range(NT):
                        pt, pn = nps()
                        for sc in range(SC):
                            P.op("pe", lambda e, pt=pt, sc=sc, tt=tt: e.matmul(
                                pt[:, 0:FW], SelGT[:, sc * TL + tt * 128:sc * TL + (tt + 1) * 128], Zf[:, sc * FW:(sc + 1) * FW],
                                start=(sc == 0), stop=(sc == SC - 1)), ["SelU", "Zf"], [pn])
                        ya = yacc[:, tt * D + fc * FW:tt * D + (fc + 1) * FW]
                        if ex_ == 0:
                            P.op("dve", lambda e, pt=pt, ya=ya: e.tensor_copy(out=ya, in_=pt[:, 0:FW]), [pn], ["yacc"])
                        else:
                            P.op("dve", lambda e, pt=pt, ya=ya: e.tensor_tensor(out=ya, in0=ya, in1=pt[:, 0:FW], op=ALU.add),
                                 [pn, "yacc"], ["yacc"])
            P.fence()
            o = NT * D
            xc = [carve(FW) for _ in range(2)]
            pc = [[carve(FW) for _ in range(2)] for _ in range(3)]
            npc = [0]

            def load_chunk(which, src_row, fc, rs):
                b = npc[0] % 2
                t = pc[which][b]
                nm = "pc%d_%d" % (which, b)
                bcast_row(t, src_row[0:1, fc * FW:(fc + 1) * FW], rs, [nm])
                return t, nm
            for tt in range(NT):
                ya = yacc[:, tt * D:(tt + 1) * D]
                for fc in range(NF):
                    xb = npc[0] % 2
                    P.dma("sync", lambda e, tt=tt, fc=fc, xb=xb: e.dma_start(
                        out=xc[xb], in_=x1_d[tt * 128:(tt + 1) * 128, fc * FW:(fc + 1) * FW]), ["x1_d"], ["xc%d" % xb])
                    t, nm = load_chunk(0, ada_d[0:1, 5 * D:6 * D], fc, ["ada_d"])
                    npc[0] += 1
                    yc = ya[:, fc * FW:(fc + 1) * FW]
                    P.op("dve", lambda e, yc=yc, t=t: e.tensor_tensor(out=yc, in0=yc, in1=t, op=ALU.mult), ["yacc", nm], ["yacc"])
                    P.op("act", lambda e, xb=xb: e.activation(out=xc[xb], in_=xc[xb], func=AF.Copy, scale=ALPHA_DN),
                         ["xc%d" % xb], ["xc%d" % xb])
                    P.op("dve", lambda e, yc=yc, xb=xb: e.tensor_tensor(out=yc, in0=yc, in1=xc[xb], op=ALU.add),
                         ["yacc", "xc%d" % xb], ["yacc"])
                layer_norm(ya, ya, None, None, ["yacc"], ["yacc"])
                for fc in range(NF):
                    tg, ng = load_chunk(1, ln2_g, fc, [])
                    tb, nb_ = load_chunk(2, ln2_b, fc, [])
                    npc[0] += 1
                    yc = ya[:, fc * FW:(fc + 1) * FW]
                    P.op("dve", lambda e, yc=yc, tg=tg: e.tensor_tensor(out=yc, in0=yc, in1=tg, op=ALU.mult), ["yacc", ng], ["yacc"])
                    P.op("dve", lambda e, yc=yc, tb=tb: e.tensor_tensor(out=yc, in0=yc, in1=tb, op=ALU.add), ["yacc", nb_], ["yacc"])
                P.dma("sync", lambda e, tt=tt, ya=ya: e.dma_start(out=out_d[tt * 128:(tt + 1) * 128, :], in_=ya), ["yacc"], ["out"])

        if stop_after in ("1a", "1b"):
            ch0 = 0 if stop_after == "1a" else G
            for tt in range(NT):
                P.dma("sync", lambda e, tt=tt: e.dma_start(out=out_d[tt * 128:(tt + 1) * 128, 0:128],
                                                          in_=YT.bitcast(F32)[:, ch0 * TL + tt * 128:ch0 * TL + (tt + 1) * 128]),
                      ["YT"], ["out"])
        if stop_after == "1c":
            xt = carve(D)
            for tt in range(NT):
                P.dma("sync", lambda e, tt=tt: e.dma_start(out=xt, in_=x1_d[tt * 128:(tt + 1) * 128, :]), ["x1_d"], ["xt"])
                P.dma("sync", lambda e, tt=tt: e.dma_start(out=out_d[tt * 128:(tt + 1) * 128, :], in_=xt), ["xt"], ["out"])
        P.fence()
        with nc.Block() as block:
            @block.sync
            def _(e):
                P.emit("sync", e)

            @block.tensor
            def _(e):
                P.emit("pe", e)

            @block.scalar
            def _(e):
                P.emit("act", e)

            @block.vector
            def _(e):
                P.emit("dve", e)

            @block.gpsimd
            def _(e):
                P.emit("pool", e)
    return nc


def host_inputs(D, inp):
    KC = D // 128
    G = H = D // 256
    FW = min(512, D)
    NF = D // FW
    KG = min(4, KC)
    NKG = KC // KG
    f = lambda a: np.ascontiguousarray(np.asarray(a, dtype=np.float32))
    x = f(inp["x"])[0]
    c = f(inp["c"])[0]
    w_exp1 = np.asarray(inp["w_exp1"], dtype=np.float32)[0]
    w_exp2 = np.asarray(inp["w_exp2"], dtype=np.float32)[0]
    b_exp1 = f(inp["b_exp1"])[0]
    b_exp2 = f(inp["b_exp2"])[0]
    slopes = [2.0 ** (-8.0 * (h + 1) / H) for h in range(H)]
    ab = np.zeros((128, H, 3, 2, 128), np.float32)
    kk = np.arange(128)[:, None]
    qq = np.arange(128)[None, :]
    for h in range(H):
        for bi, (w, d) in enumerate(BR):
            ab[:, h, bi, 0, :] = np.where(kk >= qq, -slopes[h] * d * (qq + 128 - kk), NEG)
            ab[:, h, bi, 1, :] = np.where(qq - kk >= 0, -slopes[h] * d * (qq - kk), NEG)
    common = {
        "c_col": f(c.reshape(KC, 128).T),
        "w_ada": f(inp["w_ada"])[0], "b_ada": f(inp["b_ada"])[0].reshape(1, -1),
        "w_in": f(inp["w_in"])[0],
        "sgu_g": f(inp["sgu_ln_g"])[0].reshape(1, -1), "sgu_b": f(inp["sgu_ln_b"])[0].reshape(1, -1),
        "wspT": f(np.transpose(f(inp["w_spatial"])[0], (2, 0, 1)).reshape(128, G * 128)),
        "tril": f((np.arange(128)[:, None] <= np.arange(128)[None, :]).astype(np.float32)),
        "bsp": f(inp["b_spatial"])[0].reshape(1, -1),
        "w_o": f(inp["w_o"])[0],
        "ln1_g": f(inp["ln1_g"])[0].reshape(1, -1), "ln1_b": f(inp["ln1_b"])[0].reshape(1, -1),
        "w_r": f(f(inp["w_router"])[0].reshape(KC, 128, NE).transpose(1, 0, 2).reshape(128, KC * NE)),
        "b_r": f(inp["b_router"])[0].reshape(1, -1),
        "ln2_g": f(inp["ln2_g"])[0].reshape(1, -1), "ln2_b": f(inp["ln2_b"])[0].reshape(1, -1),
        "abias": ab.reshape(128, H * 6 * 128), "ident": np.eye(128, dtype=np.float32),
        "iota": f(np.broadcast_to(np.arange(CAP, dtype=np.float32), (128, CAP))),
        "umat": f((np.arange(128)[:, None] < np.arange(128)[None, :]).astype(np.float32)),
        "w1": f(w_exp1.reshape(NE, KC, 128, 2 * KC, 128).transpose(0, 3, 2, 1, 4).reshape(NE * 2 * KC * 128, KC * 128)),
        "b1c": f(b_exp1.reshape(NE, 2 * KC, 128).transpose(2, 0, 1).reshape(128, NE * 2 * KC)),
        "w2": f(w_exp2.reshape(NE, NKG, KG, 128, NF, FW).transpose(0, 4, 1, 3, 2, 5).reshape(NE * NF * NKG * 128, KG * FW)),
        "b2": f(b_exp2.reshape(1, NE * D)),
    }
    maps = []
    p = np.arange(128)
    for cidx in range(NCORE):
        m = dict(common)
        xe = np.zeros((TE, D), np.float32)
        lo = cidx * TL - HALO
        src0 = max(lo, 0)
        xe[src0 - lo:] = x[src0:cidx * TL + TL]
        m["xe"] = xe
        kv = np.zeros((128, 32), np.float32)
        first_valid = HALO - cidx * TL
        for ch in range(24):
            kv[:, ch] = np.where(ch * 128 + p >= first_valid, 0.0, NEG)
        for ch in range(6):
            kv[:, 24 + ch] = np.where((ch * 128 + p) * 4 >= first_valid, 0.0, NEG)
        for ch in range(2):
            kv[:, 30 + ch] = np.where((ch * 128 + p) * 16 >= first_valid, 0.0, NEG)
        m["kvalid"] = kv
        maps.append(m)
    return maps


_NC_CACHE = {}


def run(D, inp, stop_after=None):
    key = (D, stop_after)
    if key not in _NC_CACHE:
        _NC_CACHE[key] = build(D, stop_after=stop_after)
    nc = _NC_CACHE[key]
    maps = host_inputs(D, inp)
    names = set()
    for alloc in nc.allocations:
        if isinstance(alloc, mybir.MemoryLocationSet) and alloc.kind == "ExternalInput":
            names.add(alloc.memorylocations[0].name)
    maps = [{k: v for k, v in m.items() if k in names} for m in maps]
    return run_bass_kernel_spmd(nc, maps, core_ids=list(range(NCORE)))


def kernel(**inputs):
    D = int(np.asarray(inputs["x"]).shape[-1])
    res = run(D, inputs)
    out = np.concatenate([r["out"] for r in res.results], axis=0)
    return out.reshape(1, S, D).astype(np.float32)
```

```python
import numpy as np
import concourse.bass as bass
import concourse.mybir as mybir
from concourse.bass_utils import run_bass_kernel_spmd

F32 = mybir.dt.float32
F32R = mybir.dt.float32r
AF = mybir.ActivationFunctionType
ALU = mybir.AluOpType
AX = mybir.AxisListType

NCORE = 8
S = 8192
TL = S // NCORE
HALO = 2048
TE = TL + HALO
NE = 32
CAP = 384
EPS = 1e-5
BR = ((128, 1), (512, 4), (2048, 16))
NEG = -30000.0
ENGS = ("pe", "act", "dve", "pool", "sync")


class Prog:
    def __init__(self, sems, dma_sems):
        self.sems = sems
        self.dma_sems = dma_sems
        self.cnt = {k: 0 for k in sems}
        self.dcnt = [0] * len(dma_sems)
        self.drr = 0
        self.lastw = {}
        self.readers = {}
        self.seen = {e: {} for e in ENGS}
        self.ops = {e: [] for e in ENGS}

    def _deps(self, eng, reads, writes):
        deps = {}

        def add(tok):
            k, v = tok
            if deps.get(k, 0) < v:
                deps[k] = v
        for r in reads:
            if r in self.lastw:
                add(self.lastw[r])
        for w in writes:
            if w in self.lastw:
                add(self.lastw[w])
            for t in self.readers.get(w, ()):
                add(t)
        out = []
        for k, v in deps.items():
            if k == eng and eng == "pe":
                continue
            if self.seen[eng].get(k, 0) >= v:
                continue
            self.seen[eng][k] = v
            out.append((k, v))
        return out

    def _commit(self, tok, reads, writes):
        for r in reads:
            self.readers.setdefault(r, []).append(tok)
        for w in writes:
            self.lastw[w] = tok
            self.readers[w] = []

    def op(self, eng, fn, reads=(), writes=()):
        waits = self._deps(eng, reads, writes)
        self.cnt[eng] += 1
        self.ops[eng].append((waits, fn, (eng, 1)))
        self._commit((eng, self.cnt[eng]), reads, writes)

    def dma(self, q, fn, reads=(), writes=()):
        k = self.drr
        self.drr = (self.drr + 1) % len(self.dma_sems)
        waits = self._deps(q, reads, writes)
        key = ("d", k)
        if self.dcnt[k] > 0 and self.seen[q].get(key, 0) < self.dcnt[k]:
            self.seen[q][key] = self.dcnt[k]
            waits.append((key, self.dcnt[k]))
        self.dcnt[k] += 16
        self.ops[q].append((waits, fn, (key, 16)))
        self._commit((key, self.dcnt[k]), reads, writes)

    def fence(self):
        cur = [(k, v) for k, v in self.cnt.items() if v > 0]
        cur += [(("d", k), v) for k, v in enumerate(self.dcnt) if v > 0]
        for eng in ENGS:
            waits = []
            for k, v in cur:
                if k == eng:
                    continue
                if self.seen[eng].get(k, 0) < v:
                    self.seen[eng][k] = v
                    waits.append((k, v))
            if waits:
                self.ops[eng].append((waits, None, None))

    def _sem(self, key):
        if isinstance(key, tuple):
            return self.dma_sems[key[1]]
        return self.sems[key]

    def emit(self, eng, e):
        for waits, fn, inc in self.ops[eng]:
            for k, v in waits:
                e.wait_ge(self._sem(k), v)
            if fn is not None:
                fn(e).then_inc(self._sem(inc[0]), inc[1])


def build(D, stop_after=None):
    KC = D // 128
    G = H = D // 256
    DIN = 5 * D // 2
    NADA = 6 * D
    FW = min(512, D)
    NF = D // FW
    KG = min(4, KC)
    NKG = KC // KG
    BT = 256
    NB = TE // BT
    HB = HALO // BT
    C = CAP
    SC = C // 128
    NT = TL // 128
    OFF_U, OFF_V, OFF_Q, OFF_K, OFF_VA = 0, D // 2, D, D + D // 2, 2 * D
    ALPHA_DN = 2.0 ** 0.25
    ARN = 20 * 1024
    ARRN = 29 * 1024 + 512
    KX = min(2, KC)

    nc = bass.Bass("TRN2", target_bir_lowering=False)

    def din(name, shape):
        return nc.dram_tensor(name, list(shape), F32, kind="ExternalInput").ap()

    xe = din("xe", [TE, D])
    c_col = din("c_col", [128, KC])
    w_ada = din("w_ada", [D, NADA])
    b_ada = din("b_ada", [1, NADA])
    w_in = din("w_in", [D, DIN])
    sgu_g = din("sgu_g", [1, D // 2])
    sgu_b = din("sgu_b", [1, D // 2])
    wspT = din("wspT", [128, G * 128])
    tril = din("tril", [128, 128])
    bsp = din("bsp", [1, G * 128])
    w_o = din("w_o", [D, D])
    ln1_g = din("ln1_g", [1, D])
    ln1_b = din("ln1_b", [1, D])
    w_r = din("w_r", [128, KC * NE])
    b_r = din("b_r", [1, NE])
    if stop_after is None:
        w1 = din("w1", [NE * 2 * KC * 128, KC * 128])
        b1c = din("b1c", [128, NE * 2 * KC])
        w2 = din("w2", [NE * NF * NKG * 128, KG * FW])
        b2 = din("b2", [1, NE * D])
        ln2_g = din("ln2_g", [1, D])
        ln2_b = din("ln2_b", [1, D])
    abias = din("abias", [128, H * 6 * 128])
    kvalid = din("kvalid", [128, 32])
    ident_d = din("ident", [128, 128])
    iota_d = din("iota", [128, C])
    umat_d = din("umat", [128, 128])
    out_d = nc.dram_tensor("out", [TL, D], F32, kind="ExternalOutput").ap()

    ada_d = nc.dram_tensor("ada_d", [1, NADA], F32).ap()
    qkvT = nc.dram_tensor("qkvT", [3 * H * 128, TE], F32).ap()
    x1_d = nc.dram_tensor("x1_d", [TL, D], F32).ap()
    x1T_d = nc.dram_tensor("x1T_d", [KC * 128, TL], F32).ap()

    import contextlib
    es = contextlib.ExitStack()
    with es:
        def sb(name, shape, dt=F32):
            return es.enter_context(nc.sbuf_tensor(name, list(shape), dt))

        def sem(name):
            return es.enter_context(nc.semaphore(name))

        sems = {k: sem("s_" + k) for k in ("pe", "act", "dve", "pool")}
        dma_sems = [sem("d%d" % i) for i in range(12)]
        P = Prog(sems, dma_sems)
        ps = [es.enter_context(nc.psum_tensor("ps%d" % i, [128, 512], F32)) for i in range(8)]
        psn = [0]

        def nps():
            i = psn[0]
            psn[0] = (i + 1) % 8
            return ps[i], "ps%d" % i

        ident = sb("ident_sb", [128, 128])
        ones = sb("ones", [128, 128])
        umat = sb("umat_sb", [128, 128])
        iota = sb("iota_sb", [128, C])
        kval = sb("kval", [128, 32])
        adac = sb("adac", [128, 4 * KC])
        ccol = sb("ccol", [128, KC])
        csil = sb("csil", [128, KC])
        Gt = sb("Gt", [128, NT * NE])
        Mk = sb("Mk", [128, NT * NE])
        pos = sb("pos", [128, NT * NE])
        st6 = sb("st6", [128, 48])
        AR = sb("arena", [128, ARN])
        ARR = sb("arenar", [128, ARRN], F32R)
        P.dma("sync", lambda e: e.dma_start(out=ident[:], in_=ident_d), [], ["ident"])
        P.dma("sync", lambda e: e.dma_start(out=umat[:], in_=umat_d), [], ["umat"])
        P.dma("sync", lambda e: e.dma_start(out=iota[:], in_=iota_d), [], ["iota"])
        P.dma("sync", lambda e: e.dma_start(out=kval[:], in_=kvalid), [], ["kval"])
        P.op("dve", lambda e: e.memset(ones[:], 1.0), [], ["ones"])

        o = 0
        orr = 0

        def carve(n):
            nonlocal o
            a = AR[:, o:o + n]
            o += n
            assert o <= ARN, ("AR", o)
            return a

        def carver(n):
            nonlocal orr
            a = ARR[:, orr:orr + n]
            orr += n
            assert orr <= ARRN, ("ARR", orr)
            return a

        def bcast_row(dst, src_row_ap, res_r, res_w):
            P.dma("pool", lambda e: e.dma_start(out=dst, in_=src_row_ap.partition_broadcast(128)), res_r, res_w)

        def layer_norm(src, dst, gb, bb, rs, ws):
            for fc in range(NF):
                P.op("dve", lambda e, fc=fc: e.bn_stats(out=st6[:, fc * 6:(fc + 1) * 6], in_=src[:, fc * FW:(fc + 1) * FW]),
                     rs, ["st6"])
            P.op("dve", lambda e: e.bn_aggr(out=st6[:, 32:34], in_=st6[:, 0:NF * 6]), ["st6"], ["st6"])
            P.op("dve", lambda e: e.tensor_scalar(out=st6[:, 34:35], in0=st6[:, 33:34], scalar1=EPS, scalar2=None,
                                                 op0=ALU.add), ["st6"], ["st6"])
            P.op("act", lambda e: e.activation(out=st6[:, 35:36], in_=st6[:, 34:35], func=AF.Sqrt), ["st6"], ["st6"])
            P.op("dve", lambda e: e.reciprocal(out=st6[:, 36:37], in_=st6[:, 35:36]), ["st6"], ["st6"])
            P.op("dve", lambda e: e.tensor_scalar(out=dst, in0=src, scalar1=st6[:, 32:33], scalar2=st6[:, 36:37],
                                                 op0=ALU.subtract, op1=ALU.mult), list(rs) + ["st6"], ws)
            if gb is not None:
                P.op("dve", lambda e: e.tensor_tensor(out=dst, in0=dst, in1=gb, op=ALU.mult), list(ws) + ["lnp"], ws)
                P.op("dve", lambda e: e.tensor_tensor(out=dst, in0=dst, in1=bb, op=ALU.add), list(ws) + ["lnp"], ws)

        P.dma("sync", lambda e: e.dma_start(out=ccol[:], in_=c_col), [], ["ccol"])
        P.op("act", lambda e: e.activation(out=csil[:], in_=ccol[:], func=AF.Silu), ["ccol"], ["csil"])
        was = [carve(512) for _ in range(4)]
        brc = [carve(512) for _ in range(2)]
        arc = [carve(512) for _ in range(2)]
        rowb = carve(D)
        AW = 512
        for j in range(NADA // AW):
            pt, pn = nps()
            jb = j % 2
            P.dma("sync", lambda e, j=j, jb=jb: e.dma_start(out=brc[jb][0:1, :], in_=b_ada[0:1, j * AW:(j + 1) * AW]),
                  [], ["brc%d" % jb])
            for kc in range(KC):
                b = (j * KC + kc) % 4
                P.dma("sync", lambda e, b=b, kc=kc, j=j: e.dma_start(
                    out=was[b], in_=w_ada[kc * 128:(kc + 1) * 128, j * AW:(j + 1) * AW]), [], ["wa%d" % b])
                P.op("pe", lambda e, b=b, kc=kc, pt=pt: e.matmul(
                    pt[0:1, 0:AW], csil[:, kc:kc + 1], was[b], start=(kc == 0), stop=(kc == KC - 1)),
                    ["csil", "wa%d" % b], [pn])
            P.op("dve", lambda e, pt=pt, jb=jb: e.tensor_tensor(
                out=arc[jb][0:1, :], in0=pt[0:1, 0:AW], in1=brc[jb][0:1, :], op=ALU.add),
                [pn, "brc%d" % jb], ["arc%d" % jb])
            P.dma("sync", lambda e, j=j, jb=jb: e.dma_start(out=ada_d[0:1, j * AW:(j + 1) * AW], in_=arc[jb][0:1, :]),
                  ["arc%d" % jb], ["ada_d"])
        pt, pn = nps()
        for vi, v in enumerate((0, 1, 3, 4)):
            P.dma("sync", lambda e, v=v: e.dma_start(out=rowb[0:1, :], in_=ada_d[0:1, v * D:(v + 1) * D]),
                  ["ada_d"], ["rowb"])
            for kc in range(KC):
                P.op("pe", lambda e, pt=pt, vi=vi, kc=kc: e.matmul(
                    pt[:, vi * KC + kc:vi * KC + kc + 1], rowb[0:1, kc * 128:(kc + 1) * 128],
                    ones[0:1, 0:1], start=True, stop=True), ["rowb", "ones"], [pn])
        for vi in range(4):
            P.op("dve", lambda e, pt=pt, vi=vi: e.tensor_scalar(
                out=adac[:, vi * KC:(vi + 1) * KC], in0=pt[:, vi * KC:(vi + 1) * KC],
                scalar1=(1.0 if vi in (1, 3) else 0.0), scalar2=None, op0=ALU.add), [pn], ["adac"])
        sh1 = lambda kc: adac[:, 0 * KC + kc:0 * KC + kc + 1]
        sc1 = lambda kc: adac[:, 1 * KC + kc:1 * KC + kc + 1]
        sh2 = lambda kc: adac[:, 2 * KC + kc:2 * KC + kc + 1]
        sc2 = lambda kc: adac[:, 3 * KC + kc:3 * KC + kc + 1]
        P.fence()

        o = 0
        orr = 0
        YT = carver(KC * TL)
        hT = carver(KC * BT)
        wt = [carver(KC * 128) for _ in range(2)]
        wv = carver(KC * 128)
        xt = carve(D)
        UT = carve(G * BT)
        stmp = carve(128)
        sgb = carve(D // 2)
        sbb = carve(D // 2)
        bspb = carve(G * 128)
        WT = carve(G * 128)
        trl = carve(128)
        vg = carve(128)
        vln = carve(128)
        stg = [carve(BT) for _ in range(4)]
        bcast_row(sgb, sgu_g, [], ["sgb"])
        bcast_row(sbb, sgu_b, [], ["sbb"])
        bcast_row(bspb, bsp, [], ["bspb"])
        P.dma("sync", lambda e: e.dma_start(out=WT, in_=wspT), [], ["WT"])
        P.dma("sync", lambda e: e.dma_start(out=trl, in_=tril), [], ["trl"])
        for g in range(G):
            P.op("dve", lambda e, g=g: e.tensor_tensor(out=WT[:, g * 128:(g + 1) * 128], in0=WT[:, g * 128:(g + 1) * 128],
                                                    in1=trl, op=ALU.mult), ["WT", "trl"], ["WT"])
        w_in_v = w_in.rearrange("(kc p) n -> p kc n", p=128)
        nstg = [0]
        nwt = [0]
        for blk in range(NB):
            local = blk >= HB
            lb = blk - HB
            for tt in range(BT // 128):
                r0 = blk * BT + tt * 128
                P.dma("sync", lambda e, r0=r0: e.dma_start(out=xt, in_=xe[r0:r0 + 128, :]), [], ["xt"])
                for kc in range(KC):
                    if kc % 4 == 0:
                        pt, pn = nps()
                    P.op("pe", lambda e, pt=pt, kc=kc: e.transpose(
                        pt[:, (kc % 4) * 128:(kc % 4 + 1) * 128], xt[:, kc * 128:(kc + 1) * 128], ident[:]),
                        ["xt", "ident"], [pn])
                    P.op("act", lambda e, pt=pt, kc=kc, tt=tt: e.activation(
                        out=hT[:, kc * BT + tt * 128:kc * BT + (tt + 1) * 128],
                        in_=pt[:, (kc % 4) * 128:(kc % 4 + 1) * 128], func=AF.Identity,
                        scale=sc1(kc), bias=sh1(kc)), [pn, "adac"], ["hT"])
            chunks = []
            if local:
                chunks += [("u", g) for g in range(G)] + [("q", h) for h in range(H)]
            chunks += [("k", h) for h in range(H)] + [("v", h) for h in range(H)]
            for kind, idx in chunks:
                col0 = {"u": OFF_U, "q": OFF_Q, "k": OFF_K, "v": OFF_VA}[kind] + idx * 128
                b = nwt[0] % 2
                nwt[0] += 1
                P.dma("pool", lambda e, b=b, col0=col0: e.dma_start(
                    out=wt[b].rearrange("p (kc n) -> p kc n", kc=KC), in_=w_in_v[:, :, col0:col0 + 128]),
                    [], ["wt%d" % b])
                pt, pn = nps()
                for kc in range(KC):
                    P.op("pe", lambda e, pt=pt, b=b, kc=kc: e.matmul(
                        pt[:, 0:BT], wt[b][:, kc * 128:(kc + 1) * 128], hT[:, kc * BT:(kc + 1) * BT],
                        start=(kc == 0), stop=(kc == KC - 1)), ["wt%d" % b, "hT"], [pn])
                if kind == "u":
                    P.op("act", lambda e, pt=pt, idx=idx: e.activation(
                        out=UT[:, idx * BT:(idx + 1) * BT], in_=pt[:, 0:BT], func=AF.Gelu), [pn], ["UT"])
                else:
                    sgi = nstg[0] % 4
                    nstg[0] += 1
                    scl = (128.0 ** -0.5) if kind == "q" else 1.0
                    P.op("act", lambda e, pt=pt, sgi=sgi, scl=scl: e.activation(
                        out=stg[sgi], in_=pt[:, 0:BT], func=AF.Copy, scale=scl), [pn], ["stg%d" % sgi])
                    row0 = ({"q": 0, "k": H, "v": 2 * H}[kind] + idx) * 128
                    P.dma("sync", lambda e, sgi=sgi, row0=row0, blk=blk: e.dma_start(
                        out=qkvT[row0:row0 + 128, blk * BT:(blk + 1) * BT], in_=stg[sgi]),
                        ["stg%d" % sgi], ["qkvT"])
            if not local:
                continue
            for g in range(G):
                P.dma("pool", lambda e, g=g: e.dma_start(
                    out=wv.rearrange("p (kc n) -> p kc n", kc=KC),
                    in_=w_in_v[:, :, OFF_V + g * 128:OFF_V + (g + 1) * 128]), [], ["wv"])
                for tt in range(BT // 128):
                    pt, pn = nps()
                    for kc in range(KC):
                        P.op("pe", lambda e, pt=pt, kc=kc, tt=tt: e.matmul(
                            pt[:, 0:128], hT[:, kc * BT + tt * 128:kc * BT + (tt + 1) * 128],
                            wv[:, kc * 128:(kc + 1) * 128], start=(kc == 0), stop=(kc == KC - 1)),
                            ["wv", "hT"], [pn])
                    P.op("act", lambda e, pt=pt: e.activation(out=vg, in_=pt[:, 0:128], func=AF.Gelu), [pn], ["vg"])
                    P.op("dve", lambda e: e.bn_stats(out=st6[:, 0:6], in_=vg), ["vg"], ["st6"])
                    P.op("dve", lambda e: e.bn_aggr(out=st6[:, 32:34], in_=st6[:, 0:6]), ["st6"], ["st6"])
                    P.op("dve", lambda e: e.tensor_scalar(out=st6[:, 34:35], in0=st6[:, 33:34], scalar1=EPS,
                                                         scalar2=None, op0=ALU.add), ["st6"], ["st6"])
                    P.op("act", lambda e: e.activation(out=st6[:, 35:36], in_=st6[:, 34:35], func=AF.Sqrt),
                         ["st6"], ["st6"])
                    P.op("dve", lambda e: e.reciprocal(out=st6[:, 36:37], in_=st6[:, 35:36]), ["st6"], ["st6"])
                    P.op("dve", lambda e: e.tensor_scalar(out=vln, in0=vg, scalar1=st6[:, 32:33], scalar2=st6[:, 36:37],
                                                         op0=ALU.subtract, op1=ALU.mult), ["vg", "st6"], ["vln"])
                    P.op("dve", lambda e, g=g: e.tensor_tensor(out=vln, in0=vln, in1=sgb[:, g * 128:(g + 1) * 128],
                                                            op=ALU.mult), ["vln", "sgb"], ["vln"])
                    P.op("dve", lambda e, g=g: e.tensor_tensor(out=vln, in0=vln, in1=sbb[:, g * 128:(g + 1) * 128],
                                                            op=ALU.add), ["vln", "sbb"], ["vln"])
                    p2, p2n = nps()
                    P.op("pe", lambda e, p2=p2, g=g: e.matmul(p2[:, 0:128], vln, WT[:, g * 128:(g + 1) * 128],
                                                           start=True, stop=True), ["vln", "WT"], [p2n])
                    P.op("dve", lambda e, p2=p2, g=g: e.tensor_tensor(out=stmp, in0=p2[:, 0:128],
                                                                   in1=bspb[:, g * 128:(g + 1) * 128], op=ALU.add),
                         [p2n, "bspb"], ["stmp"])
                    tok0 = lb * BT + tt * 128
                    P.op("dve", lambda e, g=g, tt=tt, tok0=tok0: e.tensor_tensor(
                        out=YT[:, g * TL + tok0:g * TL + tok0 + 128], in0=stmp,
                        in1=UT[:, g * BT + tt * 128:g * BT + (tt + 1) * 128], op=ALU.mult), ["stmp", "UT"], ["YT"])
        P.fence()

        if stop_after != "1a":
            o = 0
            QTh = carve(TL)
            KTh = carve(TE)
            VTh = carve(TE)
            ABh = carve(6 * 128)
            NUM = carve(TL)
            DEN = carve(TL)
            RDN = carve(TL)
            Va = [carve(128) for _ in range(2)]
            PTt = [carve(128) for _ in range(2)]
            nva = [0]

            def sl(start, step, n):
                return slice(start, start + step * (n - 1) + 1, step)
            for h in range(H):
                P.dma("sync", lambda e, h=h: e.dma_start(out=QTh, in_=qkvT[h * 128:(h + 1) * 128, HALO:TE]), ["qkvT"], ["QTh"])
                P.dma("sync", lambda e, h=h: e.dma_start(out=KTh, in_=qkvT[(H + h) * 128:(H + h + 1) * 128, :]), ["qkvT"], ["KTh"])
                P.dma("sync", lambda e, h=h: e.dma_start(out=VTh, in_=qkvT[(2 * H + h) * 128:(2 * H + h + 1) * 128, :]), ["qkvT"], ["VTh"])
                P.dma("sync", lambda e, h=h: e.dma_start(out=ABh, in_=abias[:, h * 768:(h + 1) * 768]), [], ["ABh"])
                first = True
                for bi, (win, d) in enumerate(BR):
                    i_lo = HALO // d
                    n_loc = TL // d
                    for r in range(d):
                        for q0 in range(0, n_loc, 128):
                            nq = min(128, n_loc - q0)
                            i0 = i_lo + q0
                            qs = sl(r + d * q0, d, nq)
                            pnum, pnn = nps()
                            pden, pdn = nps()
                            for ch in range(2):
                                ik0 = i0 - 128 if ch == 0 else i0
                                nk = 128 if ch == 0 else nq
                                ks = sl(r + d * ik0, d, nk)
                                col = {1: 0, 4: 24, 16: 30}[d] + ik0 // 128
                                ab0 = (bi * 2 + ch) * 128
                                pS, psn_ = nps()
                                P.op("pe", lambda e, pS=pS, ks=ks, qs=qs, nk=nk, nq=nq: e.matmul(
                                    pS[0:nk, 0:nq], KTh[:, ks], QTh[:, qs], start=True, stop=False),
                                    ["KTh", "QTh"], [psn_])
                                P.op("pe", lambda e, pS=pS, nk=nk, nq=nq, ab0=ab0: e.matmul(
                                    pS[0:nk, 0:nq], ident[0:nk, 0:nk], ABh[0:nk, ab0:ab0 + nq], start=False, stop=True),
                                    ["ident", "ABh"], [psn_])
                                vb = nva[0] % 2
                                nva[0] += 1
                                P.op("act", lambda e, pS=pS, nk=nk, nq=nq, col=col, vb=vb: e.activation(
                                    out=PTt[vb][0:nk, 0:nq], in_=pS[0:nk, 0:nq], func=AF.Exp, bias=kval[0:nk, col:col + 1]),
                                    [psn_, "kval"], ["PT%d" % vb])
                                pV, pvn = nps()
                                P.op("pe", lambda e, pV=pV, ks=ks, nk=nk: e.transpose(pV[0:nk, 0:128], VTh[:, ks], ident[:]),
                                     ["VTh", "ident"], [pvn])
                                P.op("dve", lambda e, pV=pV, nk=nk, vb=vb: e.tensor_copy(out=Va[vb][0:nk, :], in_=pV[0:nk, 0:128]),
                                     [pvn], ["Va%d" % vb])
                                P.op("pe", lambda e, pnum=pnum, nk=nk, nq=nq, vb=vb, ch=ch: e.matmul(
                                    pnum[:, 0:nq], Va[vb][0:nk, :], PTt[vb][0:nk, 0:nq], start=(ch == 0), stop=(ch == 1)),
                                    ["Va%d" % vb, "PT%d" % vb], [pnn])
                                P.op("pe", lambda e, pden=pden, nk=nk, nq=nq, vb=vb, ch=ch: e.matmul(
                                    pden[0:1, 0:nq], ones[0:nk, 0:1], PTt[vb][0:nk, 0:nq], start=(ch == 0), stop=(ch == 1)),
                                    ["ones", "PT%d" % vb], [pdn])
                            if first:
                                P.op("dve", lambda e, pnum=pnum, qs=qs, nq=nq: e.tensor_copy(out=NUM[:, qs], in_=pnum[:, 0:nq]),
                                     [pnn], ["NUM"])
                                P.op("dve", lambda e, pden=pden, qs=qs, nq=nq: e.tensor_copy(out=DEN[0:1, qs], in_=pden[0:1, 0:nq]),
                                     [pdn], ["DEN"])
                            else:
                                P.op("dve", lambda e, pnum=pnum, qs=qs, nq=nq: e.tensor_tensor(
                                    out=NUM[:, qs], in0=NUM[:, qs], in1=pnum[:, 0:nq], op=ALU.add), [pnn, "NUM"], ["NUM"])
                                P.op("dve", lambda e, pden=pden, qs=qs, nq=nq: e.tensor_tensor(
                                    out=DEN[0:1, qs], in0=DEN[0:1, qs], in1=pden[0:1, 0:nq], op=ALU.add), [pdn, "DEN"], ["DEN"])
                    first = False
                P.op("dve", lambda e: e.reciprocal(out=RDN[0:1, :], in_=DEN[0:1, :]), ["DEN"], ["RDN"])
                for hf in range(TL // 512):
                    pb, pbn = nps()
                    P.op("pe", lambda e, pb=pb, hf=hf: e.matmul(pb[:, :], ones[0:1, 0:128], RDN[0:1, hf * 512:(hf + 1) * 512],
                                                             start=True, stop=True), ["ones", "RDN"], [pbn])
                    P.op("dve", lambda e, pb=pb, hf=hf, h=h: e.tensor_tensor(
                        out=YT[:, (G + h) * TL + hf * 512:(G + h) * TL + (hf + 1) * 512],
                        in0=NUM[:, hf * 512:(hf + 1) * 512], in1=pb[:, :], op=ALU.mult), [pbn, "NUM"], ["YT"])
            P.fence()

        if stop_after not in ("1a", "1b"):
            o = 0
            orr = KC * TL
            wo = carver(KC * FW)
            xt = carve(D)
            xs = carve(D)
            pre = carve(D)
            g1b = carve(D)
            l1g = carve(D)
            l1b = carve(D)
            h2t = carve(KC * 128)
            wr = carve(KC * NE)
            brr = carve(NE)
            lg = carve(NE)
            ex = carve(NE)
            m8 = carve(8)
            sm = carve(8)
            bcast_row(g1b, ada_d[0:1, 2 * D:3 * D], ["ada_d"], ["g1b"])
            bcast_row(l1g, ln1_g, [], ["lnp"])
            bcast_row(l1b, ln1_b, [], ["lnp"])
            P.dma("sync", lambda e: e.dma_start(out=wr, in_=w_r), [], ["wr"])
            P.dma("sync", lambda e: e.dma_start(out=brr[0:1, :], in_=b_r), [], ["brr"])
            w_o_v = w_o.rearrange("(kc p) n -> p kc n", p=128)
            for tt in range(NT):
                P.dma("sync", lambda e, tt=tt: e.dma_start(out=xt, in_=xe[HALO + tt * 128:HALO + (tt + 1) * 128, :]), [], ["xt"])
                P.op("act", lambda e: e.activation(out=xs, in_=xt, func=AF.Copy, scale=ALPHA_DN), ["xt"], ["xs"])
                for fc in range(NF):
                    P.dma("pool", lambda e, fc=fc: e.dma_start(out=wo.rearrange("p (kc n) -> p kc n", kc=KC),
                                                              in_=w_o_v[:, :, fc * FW:(fc + 1) * FW]), [], ["wo"])
                    pt, pn = nps()
                    for kc in range(KC):
                        P.op("pe", lambda e, pt=pt, kc=kc, tt=tt: e.matmul(
                            pt[:, 0:FW], YT[:, kc * TL + tt * 128:kc * TL + (tt + 1) * 128], wo[:, kc * FW:(kc + 1) * FW],
                            start=(kc == 0), stop=(kc == KC - 1)), ["YT", "wo"], [pn])
                    P.op("dve", lambda e, pt=pt, fc=fc: e.tensor_tensor(out=pre[:, fc * FW:(fc + 1) * FW], in0=pt[:, 0:FW],
                                                                      in1=g1b[:, fc * FW:(fc + 1) * FW], op=ALU.mult),
                         [pn, "g1b"], ["pre"])
                P.op("dve", lambda e: e.tensor_tensor(out=pre, in0=pre, in1=xs, op=ALU.add), ["pre", "xs"], ["pre"])
                layer_norm(pre, xs, l1g, l1b, ["pre"], ["xs"])
                P.dma("sync", lambda e, tt=tt: e.dma_start(out=x1_d[tt * 128:(tt + 1) * 128, :], in_=xs), ["xs"], ["x1_d"])
                P.dma("sync", lambda e, tt=tt: e.dma_start(
                    out=x1T_d.rearrange("(kc p) t -> p kc t", p=128)[:, :, tt * 128:(tt + 1) * 128],
                    in_=xs.rearrange("p (kc n) -> p kc n", kc=KC)), ["xs"], ["x1T_d"])
                for kc in range(KC):
                    if kc % 4 == 0:
                        pt, pn = nps()
                    P.op("pe", lambda e, pt=pt, kc=kc: e.transpose(pt[:, (kc % 4) * 128:(kc % 4 + 1) * 128],
                                                                xs[:, kc * 128:(kc + 1) * 128], ident[:]), ["xs", "ident"], [pn])
                    P.op("act", lambda e, pt=pt, kc=kc: e.activation(
                        out=h2t[:, kc * 128:(kc + 1) * 128], in_=pt[:, (kc % 4) * 128:(kc % 4 + 1) * 128],
                        func=AF.Identity, scale=sc2(kc), bias=sh2(kc)), [pn, "adac"], ["h2t"])
                pt, pn = nps()
                for kc in range(KC):
                    P.op("pe", lambda e, pt=pt, kc=kc: e.matmul(pt[:, 0:NE], h2t[:, kc * 128:(kc + 1) * 128],
                                                             wr[:, kc * NE:(kc + 1) * NE], start=(kc == 0), stop=False),
                         ["h2t", "wr"], [pn])
                P.op("pe", lambda e, pt=pt: e.matmul(pt[:, 0:NE], ones[0:1, 0:128], brr[0:1, :], start=False, stop=True),
                     ["ones", "brr"], [pn])
                mk = Mk[:, tt * NE:(tt + 1) * NE]
                P.op("dve", lambda e, pt=pt: e.tensor_copy(out=lg, in_=pt[:, 0:NE]), [pn], ["lg"])
                P.op("dve", lambda e: e.max(out=m8, in_=lg), ["lg"], ["m8"])
                P.op("dve", lambda e, mk=mk: e.tensor_scalar(out=mk, in0=lg, scalar1=m8[:, 3:4], scalar2=None, op0=ALU.is_ge),
                     ["lg", "m8"], ["Mk"])
                P.op("dve", lambda e: e.tensor_scalar(out=sm[:, 0:1], in0=m8[:, 0:1], scalar1=-1.0, scalar2=None, op0=ALU.mult),
                     ["m8"], ["sm"])
                P.op("act", lambda e: e.activation(out=ex, in_=lg, func=AF.Exp, bias=sm[:, 0:1]), ["lg", "sm"], ["ex"])
                P.op("dve", lambda e, mk=mk: e.tensor_tensor(out=ex, in0=ex, in1=mk, op=ALU.mult), ["ex", "Mk"], ["ex"])
                P.op("dve", lambda e: e.reduce_sum(out=sm[:, 1:2], in_=ex, axis=AX.X), ["ex"], ["sm"])
                P.op("dve", lambda e: e.reciprocal(out=sm[:, 2:3], in_=sm[:, 1:2]), ["sm"], ["sm"])
                P.op("dve", lambda e, tt=tt: e.tensor_scalar(out=Gt[:, tt * NE:(tt + 1) * NE], in0=ex, scalar1=sm[:, 2:3],
                                                            scalar2=None, op0=ALU.mult), ["ex", "sm"], ["Gt"])
            for tt in range(NT):
                pt, pn = nps()
                for t2 in range(tt):
                    P.op("pe", lambda e, pt=pt, t2=t2: e.matmul(pt[:, 0:NE], ones[:, :], Mk[:, t2 * NE:(t2 + 1) * NE],
                                                             start=(t2 == 0), stop=False), ["ones", "Mk"], [pn])
                P.op("pe", lambda e, pt=pt, tt=tt: e.matmul(pt[:, 0:NE], umat[:, :], Mk[:, tt * NE:(tt + 1) * NE],
                                                         start=(tt == 0), stop=True), ["umat", "Mk"], [pn])
                pp = pos[:, tt * NE:(tt + 1) * NE]
                P.op("dve", lambda e, pt=pt, pp=pp: e.tensor_scalar(out=pp, in0=pt[:, 0:NE], scalar1=1.0, scalar2=None,
                                                                  op0=ALU.add), [pn], ["pos"])
                P.op("dve", lambda e, pp=pp, tt=tt: e.tensor_tensor(out=pp, in0=pp, in1=Mk[:, tt * NE:(tt + 1) * NE],
                                                                  op=ALU.mult), ["pos", "Mk"], ["pos"])
                P.op("dve", lambda e, pp=pp: e.tensor_scalar(out=pp, in0=pp, scalar1=-1.0, scalar2=None, op0=ALU.add),
                     ["pos"], ["pos"])
            P.fence()

        if stop_after is None:
            o = 0
            orr = 0
            yacc = carve(NT * D)
            b2c = [carve(FW) for _ in range(2)]
            b1s = carve(NE * 2 * KC)
            sg = [carve(C) for _ in range(2)]
            t1 = carve(C)
            t2 = carve(C)
            t3 = carve(C)
            xsl = [carver(NT * 128) for _ in range(3)]
            XgT = carver(KC * C)
            actT = carver(KC * C)
            Zf = carver(SC * FW)
            assert NT * C == SC * TL
            Sel = carver(NT * C)
            SelGT = Sel
            NW1 = 3
            w1t = [carver(KC * 128) for _ in range(NW1)]
            w2t = [carver(KG * FW) for _ in range(2)]
            P.dma("sync", lambda e: e.dma_start(out=b1s, in_=b1c), [], ["b1s"])
            nw1 = [0]
            nw2 = [0]
            nb2 = [0]

            def issue_w1(ex_, t):
                j, half = t
                b = nw1[0] % NW1
                nw1[0] += 1
                row0 = (ex_ * 2 * KC + half * KC + j) * 128
                P.dma("pool", lambda e, b=b, row0=row0: e.dma_start(out=w1t[b], in_=w1[row0:row0 + 128, :]),
                      [], ["w1t%d" % b])
                return b

            def issue_w2(ex_, t):
                fc, kg = t
                b = nw2[0] % 2
                nw2[0] += 1
                row0 = ((ex_ * NF + fc) * NKG + kg) * 128
                P.dma("pool", lambda e, b=b, row0=row0: e.dma_start(out=w2t[b], in_=w2[row0:row0 + 128, :]),
                      [], ["w2t%d" % b])
                return b

            def issue_x(kc):
                b = kc % 3
                P.dma("pool", lambda e, b=b, kc=kc: e.dma_start(out=xsl[b], in_=x1T_d[kc * 128:(kc + 1) * 128, :]),
                      ["x1T_d"], ["xsl%d" % b])
                return b
            w1_tiles = [(j, half) for j in range(KC) for half in range(2)]
            w2_tiles = [(fc, kg) for fc in range(NF) for kg in range(NKG)]
            for ex_ in range(NE):
                for tt in range(NT):
                    P.op("dve", lambda e, tt=tt, ex_=ex_: e.tensor_scalar(
                        out=Sel[:, tt * C:(tt + 1) * C], in0=iota[:, :], scalar1=pos[:, tt * NE + ex_:tt * NE + ex_ + 1],
                        scalar2=None, op0=ALU.is_equal), ["iota", "pos"], ["SelU"])
                xq = [issue_x(kc) for kc in range(min(3, KC))]
                w1q = [issue_w1(ex_, t) for t in w1_tiles[:NW1]]
                for kc in range(KC):
                    b = xq[kc]
                    pt, pn = nps()
                    for tt in range(NT):
                        P.op("pe", lambda e, pt=pt, tt=tt, b=b: e.matmul(
                            pt[:, 0:C], xsl[b][:, tt * 128:(tt + 1) * 128],
                            Sel[:, tt * C:(tt + 1) * C], start=(tt == 0), stop=(tt == NT - 1)), ["xsl%d" % b, "SelU"], [pn])
                    if kc + 3 < KC:
                        xq.append(issue_x(kc + 3))
                    P.op("act", lambda e, pt=pt, kc=kc: e.activation(out=XgT[:, kc * C:(kc + 1) * C], in_=pt[:, 0:C],
                                                                   func=AF.Identity, scale=sc2(kc), bias=sh2(kc)),
                         [pn, "adac"], ["XgT"])
                w2q = [issue_w2(ex_, t) for t in w2_tiles[:2]]
                for tt in range(NT):
                    sb_ = tt % 2
                    P.op("dve", lambda e, tt=tt, ex_=ex_, sb_=sb_: e.tensor_scalar(
                        out=sg[sb_], in0=iota[:, :], scalar1=pos[:, tt * NE + ex_:tt * NE + ex_ + 1],
                        scalar2=Gt[:, tt * NE + ex_:tt * NE + ex_ + 1], op0=ALU.is_equal, op1=ALU.mult),
                        ["iota", "pos", "Gt"], ["sg%d" % sb_])
                    pt, pn = nps()
                    for sc in range(SC):
                        P.op("pe", lambda e, pt=pt, sc=sc, sb_=sb_: e.transpose(
                            pt[:, sc * 128:(sc + 1) * 128], sg[sb_][:, sc * 128:(sc + 1) * 128], ident[:]),
                            ["sg%d" % sb_, "ident"], [pn])
                        P.op("act", lambda e, pt=pt, sc=sc, tt=tt: e.activation(
                            out=SelGT[:, sc * TL + tt * 128:sc * TL + (tt + 1) * 128], in_=pt[:, sc * 128:(sc + 1) * 128],
                            func=AF.Copy), [pn], ["SelU"])
                pgl = []
                for ti, (j, half) in enumerate(w1_tiles):
                    b = w1q[ti]
                    pt, pn = nps()
                    for kc in range(KC):
                        P.op("pe", lambda e, pt=pt, b=b, kc=kc: e.matmul(
                            pt[:, 0:C], w1t[b][:, kc * 128:(kc + 1) * 128], XgT[:, kc * C:(kc + 1) * C],
                            start=(kc == 0), stop=(kc == KC - 1)), ["w1t%d" % b, "XgT"], [pn])
                    if ti + NW1 < len(w1_tiles):
                        w1q.append(issue_w1(ex_, w1_tiles[ti + NW1]))
                    pgl.append((pt, pn))
                    if half == 0:
                        continue
                    (pg, pgn), (pl, pln) = pgl
                    pgl = []
                    cg = ex_ * 2 * KC + j
                    cl = ex_ * 2 * KC + KC + j
                    P.op("dve", lambda e, pg=pg, cg=cg: e.tensor_scalar(out=t1, in0=pg[:, 0:C], scalar1=b1s[:, cg:cg + 1],
                                                                       scalar2=7.0, op0=ALU.add, op1=ALU.min), [pgn, "b1s"], ["t1"])
                    P.op("dve", lambda e, pl=pl, cl=cl: e.tensor_scalar(out=t3, in0=pl[:, 0:C], scalar1=b1s[:, cl:cl + 1],
                                                                       scalar2=7.0, op0=ALU.add, op1=ALU.min), [pln, "b1s"], ["t3"])
                    P.op("act", lambda e: e.activation(out=t2, in_=t1, func=AF.Sigmoid, scale=1.702), ["t1"], ["t2"])
                    P.op("dve", lambda e: e.tensor_scalar(out=t3, in0=t3, scalar1=-7.0, scalar2=1.0, op0=ALU.max, op1=ALU.add),
                         ["t3"], ["t3"])
                    P.op("dve", lambda e: e.tensor_tensor(out=t1, in0=t1, in1=t2, op=ALU.mult), ["t1", "t2"], ["t1"])
                    P.op("dve", lambda e, j=j: e.tensor_tensor(out=actT[:, j * C:(j + 1) * C], in0=t1, in1=t3, op=ALU.mult),
                         ["t1", "t3"], ["actT"])
                for fc in range(NF):
                    bb = nb2[0] % 2
                    nb2[0] += 1
                    P.dma("sync", lambda e, ex_=ex_, fc=fc, bb=bb: e.dma_start(
                        out=b2c[bb][0:1, :], in_=b2[0:1, ex_ * D + fc * FW:ex_ * D + (fc + 1) * FW]), [], ["b2c%d" % bb])
                    pz = [nps() for _ in range(SC)]
                    for kg in range(NKG):
                        ti = fc * NKG + kg
                        b = w2q[ti]
                        for sc in range(SC):
                            for k4 in range(KG):
                                kc = kg * KG + k4
                                P.op("pe", lambda e, sc=sc, k4=k4, kc=kc, b=b, pz=pz: e.matmul(
                                    pz[sc][0][:, 0:FW], actT[:, kc * C + sc * 128:kc * C + (sc + 1) * 128],
                                    w2t[b][:, k4 * FW:(k4 + 1) * FW], start=(kc == 0), stop=False),
                                    ["actT", "w2t%d" % b], [pz[sc][1]])
                        if ti + 2 < len(w2_tiles):
                            w2q.append(issue_w2(ex_, w2_tiles[ti + 2]))
                    for sc in range(SC):
                        P.op("pe", lambda e, sc=sc, bb=bb, pz=pz: e.matmul(
                            pz[sc][0][:, 0:FW], ones[0:1, 0:128], b2c[bb][0:1, :], start=False, stop=True),
                            ["ones", "b2c%d" % bb], [pz[sc][1]])
                        P.op("act", lambda e, sc=sc, pz=pz: e.activation(out=Zf[:, sc * FW:(sc + 1) * FW], in_=pz[sc][0][:, 0:FW],
                                                                       func=AF.Copy), [pz[sc][1]], ["Zf"])
                    for tt in range(NT):
                        pt, pn = nps()
                        for sc in range(SC):
                            P.op("pe", lambda e, pt=pt, sc=sc, tt=tt: e.matmul(
                                pt[:, 0:FW], SelGT[:, sc * TL + tt * 128:sc * TL + (tt + 1) * 128], Zf[:, sc * FW:(sc + 1) * FW],
                                start=(sc == 0), stop=(sc == SC - 1)), ["SelU", "Zf"], [pn])
                        ya = yacc[:, tt * D + fc * FW:tt * D + (fc + 1) * FW]
                        if ex_ == 0:
                            P.op("dve", lambda e, pt=pt, ya=ya: e.tensor_copy(out=ya, in_=pt[:, 0:FW]), [pn], ["yacc"])
                        else:
                            P.op("dve", lambda e, pt=pt, ya=ya: e.tensor_tensor(out=ya, in0=ya, in1=pt[:, 0:FW], op=ALU.add),
                                 [pn, "yacc"], ["yacc"])
            P.fence()
            o = NT * D
            xc = [carve(FW) for _ in range(2)]
            pc = [[carve(FW) for _ in range(2)] for _ in range(3)]
            npc = [0]

            def load_chunk(which, src_row, fc, rs):
                b = npc[0] % 2
                t = pc[which][b]
                nm = "pc%d_%d" % (which, b)
                bcast_row(t, src_row[0:1, fc * FW:(fc + 1) * FW], rs, [nm])
                return t, nm
            for tt in range(NT):
                ya = yacc[:, tt * D:(tt + 1) * D]
                for fc in range(NF):
                    xb = npc[0] % 2
                    P.dma("sync", lambda e, tt=tt, fc=fc, xb=xb: e.dma_start(
                        out=xc[xb], in_=x1_d[tt * 128:(tt + 1) * 128, fc * FW:(fc + 1) * FW]), ["x1_d"], ["xc%d" % xb])
                    t, nm = load_chunk(0, ada_d[0:1, 5 * D:6 * D], fc, ["ada_d"])
                    npc[0] += 1
                    yc = ya[:, fc * FW:(fc + 1) * FW]
                    P.op("dve", lambda e, yc=yc, t=t: e.tensor_tensor(out=yc, in0=yc, in1=t, op=ALU.mult), ["yacc", nm], ["yacc"])
                    P.op("act", lambda e, xb=xb: e.activation(out=xc[xb], in_=xc[xb], func=AF.Copy, scale=ALPHA_DN),
                         ["xc%d" % xb], ["xc%d" % xb])
                    P.op("dve", lambda e, yc=yc, xb=xb: e.tensor_tensor(out=yc, in0=yc, in1=xc[xb], op=ALU.add),
                         ["yacc", "xc%d" % xb], ["yacc"])
                layer_norm(ya, ya, None, None, ["yacc"], ["yacc"])
                for fc in range(NF):
                    tg, ng = load_chunk(1, ln2_g, fc, [])
                    tb, nb_ = load_chunk(2, ln2_b, fc, [])
                    npc[0] += 1
                    yc = ya[:, fc * FW:(fc + 1) * FW]
                    P.op("dve", lambda e, yc=yc, tg=tg: e.tensor_tensor(out=yc, in0=yc, in1=tg, op=ALU.mult), ["yacc", ng], ["yacc"])
                    P.op("dve", lambda e, yc=yc, tb=tb: e.tensor_tensor(out=yc, in0=yc, in1=tb, op=ALU.add), ["yacc", nb_], ["yacc"])
                P.dma("sync", lambda e, tt=tt, ya=ya: e.dma_start(out=out_d[tt * 128:(tt + 1) * 128, :], in_=ya), ["yacc"], ["out"])

        if stop_after in ("1a", "1b"):
            ch0 = 0 if stop_after == "1a" else G
            for tt in range(NT):
                P.dma("sync", lambda e, tt=tt: e.dma_start(out=out_d[tt * 128:(tt + 1) * 128, 0:128],
                                                          in_=YT.bitcast(F32)[:, ch0 * TL + tt * 128:ch0 * TL + (tt + 1) * 128]),
                      ["YT"], ["out"])
        if stop_after == "1c":
            xt = carve(D)
            for tt in range(NT):
                P.dma("sync", lambda e, tt=tt: e.dma_start(out=xt, in_=x1_d[tt * 128:(tt + 1) * 128, :]), ["x1_d"], ["xt"])
                P.dma("sync", lambda e, tt=tt: e.dma_start(out=out_d[tt * 128:(tt + 1) * 128, :], in_=xt), ["xt"], ["out"])
        P.fence()
        with nc.Block() as block:
            @block.sync
            def _(e):
                P.emit("sync", e)

            @block.tensor
            def _(e):
                P.emit("pe", e)

            @block.scalar
            def _(e):
                P.emit("act", e)

            @block.vector
            def _(e):
                P.emit("dve", e)

            @block.gpsimd
            def _(e):
                P.emit("pool", e)
    return nc


def host_inputs(D, inp):
    KC = D // 128
    G = H = D // 256
    FW = min(512, D)
    NF = D // FW
    KG = min(4, KC)
    NKG = KC // KG
    f = lambda a: np.ascontiguousarray(np.asarray(a, dtype=np.float32))
    x = f(inp["x"])[0]
    c = f(inp["c"])[0]
    w_exp1 = np.asarray(inp["w_exp1"], dtype=np.float32)[0]
    w_exp2 = np.asarray(inp["w_exp2"], dtype=np.float32)[0]
    b_exp1 = f(inp["b_exp1"])[0]
    b_exp2 = f(inp["b_exp2"])[0]
    slopes = [2.0 ** (-8.0 * (h + 1) / H) for h in range(H)]
    ab = np.zeros((128, H, 3, 2, 128), np.float32)
    kk = np.arange(128)[:, None]
    qq = np.arange(128)[None, :]
    for h in range(H):
        for bi, (w, d) in enumerate(BR):
            ab[:, h, bi, 0, :] = np.where(kk >= qq, -slopes[h] * d * (qq + 128 - kk), NEG)
            ab[:, h, bi, 1, :] = np.where(qq - kk >= 0, -slopes[h] * d * (qq - kk), NEG)
    common = {
        "c_col": f(c.reshape(KC, 128).T),
        "w_ada": f(inp["w_ada"])[0], "b_ada": f(inp["b_ada"])[0].reshape(1, -1),
        "w_in": f(inp["w_in"])[0],
        "sgu_g": f(inp["sgu_ln_g"])[0].reshape(1, -1), "sgu_b": f(inp["sgu_ln_b"])[0].reshape(1, -1),
        "wspT": f(np.transpose(f(inp["w_spatial"])[0], (2, 0, 1)).reshape(128, G * 128)),
        "tril": f((np.arange(128)[:, None] <= np.arange(128)[None, :]).astype(np.float32)),
        "bsp": f(inp["b_spatial"])[0].reshape(1, -1),
        "w_o": f(inp["w_o"])[0],
        "ln1_g": f(inp["ln1_g"])[0].reshape(1, -1), "ln1_b": f(inp["ln1_b"])[0].reshape(1, -1),
        "w_r": f(f(inp["w_router"])[0].reshape(KC, 128, NE).transpose(1, 0, 2).reshape(128, KC * NE)),
        "b_r": f(inp["b_router"])[0].reshape(1, -1),
        "ln2_g": f(inp["ln2_g"])[0].reshape(1, -1), "ln2_b": f(inp["ln2_b"])[0].reshape(1, -1),
        "abias": ab.reshape(128, H * 6 * 128), "ident": np.eye(128, dtype=np.float32),
        "iota": f(np.broadcast_to(np.arange(CAP, dtype=np.float32), (128, CAP))),
        "umat": f((np.arange(128)[:, None] < np.arange(128)[None, :]).astype(np.float32)),
        "w1": f(w_exp1.reshape(NE, KC, 128, 2 * KC, 128).transpose(0, 3, 2, 1, 4).reshape(NE * 2 * KC * 128, KC * 128)),
        "b1c": f(b_exp1.reshape(NE, 2 * KC, 128).transpose(2, 0, 1).reshape(128, NE * 2 * KC)),
        "w2": f(w_exp2.reshape(NE, NKG, KG, 128, NF, FW).transpose(0, 4, 1, 3, 2, 5).reshape(NE * NF * NKG * 128, KG * FW)),
        "b2": f(b_exp2.reshape(1, NE * D)),
    }
    maps = []
    p = np.arange(128)
    for cidx in range(NCORE):
        m = dict(common)
        xe = np.zeros((TE, D), np.float32)
        lo = cidx * TL - HALO
        src0 = max(lo, 0)
        xe[src0 - lo:] = x[src0:cidx * TL + TL]
        m["xe"] = xe
        kv = np.zeros((128, 32), np.float32)
        first_valid = HALO - cidx * TL
        for ch in range(24):
            kv[:, ch] = np.where(ch * 128 + p >= first_valid, 0.0, NEG)
        for ch in range(6):
            kv[:, 24 + ch] = np.where((ch * 128 + p) * 4 >= first_valid, 0.0, NEG)
        for ch in range(2):
            kv[:, 30 + ch] = np.where((ch * 128 + p) * 16 >= first_valid, 0.0, NEG)
        m["kvalid"] = kv
        maps.append(m)
    return maps


_NC_CACHE = {}


def run(D, inp, stop_after=None):
    key = (D, stop_after)
    if key not in _NC_CACHE:
        _NC_CACHE[key] = build(D, stop_after=stop_after)
    nc = _NC_CACHE[key]
    maps = host_inputs(D, inp)
    names = set()
    for alloc in nc.allocations:
        if isinstance(alloc, mybir.MemoryLocationSet) and alloc.kind == "ExternalInput":
            names.add(alloc.memorylocations[0].name)
    maps = [{k: v for k, v in m.items() if k in names} for m in maps]
    return run_bass_kernel_spmd(nc, maps, core_ids=list(range(NCORE)))


def kernel(**inputs):
    D = int(np.asarray(inputs["x"]).shape[-1])
    res = run(D, inputs)
    out = np.concatenate([r["out"] for r in res.results], axis=0)
    return out.reshape(1, S, D).astype(np.float32)
```
